# Optimizing a Trainium2 kernel written in Bass

```python
import jax, jax.numpy as jnp
from jax import lax
import numpy as np

D_MODEL = 1024
BATCH = 32
SEQ = 2048
DEPTH = 4
DEC_BATCH = 16
DEC_SEQ = 2048
PAST_LEN = 128

MIX_WIDTH = D_MODEL
FOURIER_WIDTH = D_MODEL // 4
N_FGROUPS = 4
FGROUP_DIM = FOURIER_WIDTH // N_FGROUPS
HEAD_DIM = 64
ATTN_WIDTH = MIX_WIDTH - FOURIER_WIDTH
N_Q_HEADS = ATTN_WIDTH // HEAD_DIM
N_KV_HEADS = 4
Q_PER_KV = N_Q_HEADS // N_KV_HEADS
KV_WIDTH = N_KV_HEADS * HEAD_DIM
IN_WIDTH = FOURIER_WIDTH + ATTN_WIDTH + 2 * KV_WIDTH
WINDOW = 128
BLOCK = 128
ROPE_THETA = 500000.0
ROT_DIM = HEAD_DIM // 4
N_EXPERTS = 32
TOP_K = 4
D_FF = D_MODEL // 2
EXPERT_BLOCK = 512
SWIGLU_LIMIT = 7.0
SWIGLU_ALPHA = 1.702
NORM_EPS = 1e-5

kernel_name = "hymba_fnet_swa_moe_adaln_encoder"


def rms_norm(x, g):
    x32 = x.astype(jnp.float32)
    y = x32 * lax.rsqrt(jnp.mean(x32 * x32, axis=-1, keepdims=True) + NORM_EPS)
    return (y * g.astype(jnp.float32)).astype(x.dtype)


def partial_rotary(x, pos):
    half = ROT_DIM // 2
    inv_freq = jnp.power(ROPE_THETA, -jnp.arange(0, ROT_DIM, 2, dtype=jnp.float32) / ROT_DIM)
    ang = pos.astype(jnp.float32)[:, None] * inv_freq[None, :]
    cos = jnp.cos(ang)[None, :, None, :]
    sin = jnp.sin(ang)[None, :, None, :]
    xr = x[..., :ROT_DIM].astype(jnp.float32)
    x1, x2 = xr[..., :half], xr[..., half:]
    rot = jnp.concatenate([x1 * cos - x2 * sin, x2 * cos + x1 * sin], axis=-1).astype(x.dtype)
    return jnp.concatenate([rot, x[..., ROT_DIM:]], axis=-1)


def fourier_mix(f, w_fmix):
    B, S = f.shape[:2]
    fg = f.reshape(B, S, N_FGROUPS, FGROUP_DIM).astype(jnp.float32)
    spec = jnp.fft.fft2(fg, axes=(1, 3), norm="ortho").real.astype(f.dtype)
    out = jnp.einsum('bsgc,gcd->bsgd', spec, w_fmix)
    return out.reshape(B, S, FOURIER_WIDTH)


def windowed_attention(q, k, v, sinks):
    B, S = q.shape[:2]
    nb = S // BLOCK
    qb = q.reshape(B, nb, BLOCK, N_KV_HEADS, Q_PER_KV, HEAD_DIM)

    def windows(t):
        tp = jnp.pad(t, ((0, 0), (BLOCK, BLOCK), (0, 0), (0, 0)))
        tp = tp.reshape(B, nb + 2, BLOCK, N_KV_HEADS, HEAD_DIM)
        return jnp.concatenate([tp[:, :-2], tp[:, 1:-1], tp[:, 2:]], axis=2)

    kw, vw = windows(k), windows(v)
    s = jnp.einsum('bnqhgd,bnkhd->bnhgqk', qb, kw).astype(jnp.float32) * (HEAD_DIM ** -0.5)
    qpos = jnp.arange(nb)[:, None] * BLOCK + jnp.arange(BLOCK)[None, :]
    kpos = (jnp.arange(nb)[:, None] - 1) * BLOCK + jnp.arange(3 * BLOCK)[None, :]
    valid = ((jnp.abs(kpos[:, None, :] - qpos[:, :, None]) <= WINDOW)
             & (kpos >= 0)[:, None, :] & (kpos < S)[:, None, :])
    s = jnp.where(valid[None, :, None, None], s, -jnp.inf)
    sink = sinks.astype(jnp.float32).reshape(N_KV_HEADS, Q_PER_KV)[None, None, :, :, None, None]
    m = jnp.maximum(jnp.max(s, axis=-1, keepdims=True), sink)
    p = jnp.exp(s - m)
    p = (p / (jnp.sum(p, axis=-1, keepdims=True) + jnp.exp(sink - m))).astype(v.dtype)
    o = jnp.einsum('bnhgqk,bnkhd->bnqhgd', p, vw)
    return o.reshape(B, S, N_Q_HEADS * HEAD_DIM)


def moe_ffn(h, w_router, b_router, w_gu, b_gu, w_down, b_down):
    shp = h.shape
    t = h.reshape(-1, D_MODEL)
    T = t.shape[0]
    A = T * TOP_K
    NB = -(-A // EXPERT_BLOCK) + N_EXPERTS
    P = NB * EXPERT_BLOCK
    logits = (t @ w_router + b_router).astype(jnp.float32)
    vals, idx = lax.top_k(logits, TOP_K)
    wts = jax.nn.softmax(vals, axis=-1)
    flat_e = idx.reshape(-1)
    flat_tok = jnp.repeat(jnp.arange(T, dtype=jnp.int32), TOP_K)
    flat_w = wts.reshape(-1)
    order = jnp.argsort(flat_e, stable=True)
    se = flat_e[order]
    counts = jnp.zeros((N_EXPERTS,), jnp.int32).at[flat_e].add(1)
    padded = ((counts + EXPERT_BLOCK - 1) // EXPERT_BLOCK) * EXPERT_BLOCK
    start = jnp.cumsum(counts) - counts
    pad_end = jnp.cumsum(padded)
    pad_start = pad_end - padded
    dest = pad_start[se] + (jnp.arange(A, dtype=jnp.int32) - start[se])
    row_tok = jnp.zeros((P,), jnp.int32).at[dest].set(flat_tok[order])
    row_w = jnp.zeros((P,), jnp.float32).at[dest].set(flat_w[order]).astype(t.dtype)
    blk_start = jnp.arange(NB, dtype=jnp.int32) * EXPERT_BLOCK
    bexp = jnp.minimum(jnp.sum(pad_end[None, :] <= blk_start[:, None], axis=1), N_EXPERTS - 1)
    xs = t[row_tok].reshape(NB, EXPERT_BLOCK, D_MODEL)
    gu = jnp.einsum('nrd,ndf->nrf', xs, w_gu[bexp]) + b_gu[bexp][:, None, :]
    g = jnp.minimum(gu[..., :D_FF], SWIGLU_LIMIT)
    u = jnp.clip(gu[..., D_FF:], -SWIGLU_LIMIT, SWIGLU_LIMIT)
    act = (u + 1) * (g * jax.nn.sigmoid(SWIGLU_ALPHA * g))
    y = jnp.einsum('nrf,nfd->nrd', act, w_down[bexp]) + b_down[bexp][:, None, :]
    y = y.reshape(P, D_MODEL) * row_w[:, None]
    out = jnp.zeros_like(t).at[row_tok].add(y)
    return out.reshape(shp)


def encoder_layer(x, c, norm1_g, ada_w, ada_b, w_in, b_in, w_fmix, sinks, w_o, b_o,
                  norm2_g, w_router, b_router, w_gu, b_gu, w_down, b_down):
    B, S = x.shape[:2]
    mod = (jax.nn.silu(c) @ ada_w + ada_b)[:, None, :]
    sh1, sc1, g1, sh2, sc2, g2 = jnp.split(mod, 6, axis=-1)
    h = rms_norm(x, norm1_g) * (1 + sc1) + sh1
    z = h @ w_in + b_in
    f, q, k, v = jnp.split(z, [FOURIER_WIDTH, FOURIER_WIDTH + ATTN_WIDTH,
                               FOURIER_WIDTH + ATTN_WIDTH + KV_WIDTH], axis=-1)
    pos = jnp.arange(S)
    q = partial_rotary(q.reshape(B, S, N_Q_HEADS, HEAD_DIM), pos)
    k = partial_rotary(k.reshape(B, S, N_KV_HEADS, HEAD_DIM), pos)
    v = v.reshape(B, S, N_KV_HEADS, HEAD_DIM)
    mixed = jnp.concatenate([fourier_mix(f, w_fmix), windowed_attention(q, k, v, sinks)], axis=-1)
    x = x + g1 * (mixed @ w_o + b_o)
    h = rms_norm(x, norm2_g) * (1 + sc2) + sh2
    x = x + g2 * moe_ffn(h, w_router, b_router, w_gu, b_gu, w_down, b_down)
    return x


def trunk(x, c, norm1_g, ada_w, ada_b, w_in, b_in, w_fmix, sinks, w_o, b_o,
          norm2_g, w_router, b_router, w_gu, b_gu, w_down, b_down, final_g):
    for l in range(DEPTH):
        x = encoder_layer(x, c, norm1_g[l], ada_w[l], ada_b[l], w_in[l], b_in[l], w_fmix[l],
                          sinks[l], w_o[l], b_o[l], norm2_g[l], w_router[l], b_router[l],
                          w_gu[l], b_gu[l], w_down[l], b_down[l])
    return rms_norm(x, final_g)


def setup_inputs(seed: int = 0) -> dict:
    key = jax.random.key(seed)
    ks = jax.random.split(key, 24)
    f32 = jnp.float32
    nrm = lambda k, shape, s: jax.random.normal(k, shape, f32) * s
    return {
        "x_prompt": nrm(ks[0], (BATCH, SEQ, D_MODEL), 1.0),
        "x_sample": nrm(ks[1], (DEC_BATCH, DEC_SEQ, D_MODEL), 1.0),
        "c_prompt": nrm(ks[2], (BATCH, D_MODEL), 1.0),
        "c_sample": nrm(ks[3], (DEC_BATCH, D_MODEL), 1.0),
        "norm1_g": 1.0 + nrm(ks[4], (DEPTH, D_MODEL), 0.02),
        "ada_w": nrm(ks[5], (DEPTH, D_MODEL, 6 * D_MODEL), 0.5 * D_MODEL ** -0.5),
        "ada_b": nrm(ks[6], (DEPTH, 6 * D_MODEL), 0.01),
        "w_in": nrm(ks[7], (DEPTH, D_MODEL, IN_WIDTH), D_MODEL ** -0.5),
        "b_in": nrm(ks[8], (DEPTH, IN_WIDTH), 0.01),
        "w_fmix": nrm(ks[9], (DEPTH, N_FGROUPS, FGROUP_DIM, FGROUP_DIM), FGROUP_DIM ** -0.5),
        "sinks": nrm(ks[10], (DEPTH, N_Q_HEADS), 0.5),
        "w_o": nrm(ks[11], (DEPTH, MIX_WIDTH, D_MODEL), MIX_WIDTH ** -0.5),
        "b_o": nrm(ks[12], (DEPTH, D_MODEL), 0.01),
        "norm2_g": 1.0 + nrm(ks[13], (DEPTH, D_MODEL), 0.02),
        "w_router": nrm(ks[14], (DEPTH, D_MODEL, N_EXPERTS), D_MODEL ** -0.5),
        "b_router": nrm(ks[15], (DEPTH, N_EXPERTS), 0.01),
        "w_gu": nrm(ks[16], (DEPTH, N_EXPERTS, D_MODEL, 2 * D_FF), D_MODEL ** -0.5),
        "b_gu": nrm(ks[17], (DEPTH, N_EXPERTS, 2 * D_FF), 0.01),
        "w_down": nrm(ks[18], (DEPTH, N_EXPERTS, D_FF, D_MODEL), D_FF ** -0.5),
        "b_down": nrm(ks[19], (DEPTH, N_EXPERTS, D_MODEL), 0.01),
        "final_g": 1.0 + nrm(ks[20], (D_MODEL,), 0.02),
    }


def reference(x_prompt, x_sample, c_prompt, c_sample, norm1_g, ada_w, ada_b, w_in, b_in, w_fmix,
              sinks, w_o, b_o, norm2_g, w_router, b_router, w_gu, b_gu, w_down, b_down, final_g):
    y_prompt = trunk(x_prompt, c_prompt, norm1_g, ada_w, ada_b, w_in, b_in, w_fmix, sinks, w_o, b_o,
                     norm2_g, w_router, b_router, w_gu, b_gu, w_down, b_down, final_g)
    y_sample = trunk(x_sample, c_sample, norm1_g, ada_w, ada_b, w_in, b_in, w_fmix, sinks, w_o, b_o,
                     norm2_g, w_router, b_router, w_gu, b_gu, w_down, b_down, final_g)
    return (y_prompt, y_sample)
```

```python
import contextlib
import numpy as np
import ml_dtypes
import concourse.bass as bass
import concourse.mybir as mybir
from concourse.bass_utils import run_bass_kernel_spmd

F32 = mybir.dt.float32
BF16 = mybir.dt.bfloat16
I32 = mybir.dt.int32
AF = mybir.ActivationFunctionType
ALU = mybir.AluOpType

SAME_ENGINE_SYNC = True

D = 1024
SEQ = 2048
TPS = 16
NE = 32
DFF = 512
INW = 1536
EPS = 1e-5


class _Stop(Exception):
    pass


class Res:
    __slots__ = ("name", "w", "r", "excl")

    def __init__(self, name="", excl=False):
        self.name = name
        self.w = None
        self.r = []
        self.excl = excl


class Sched:
    ENGS = ("pe", "act", "dve", "pool", "sp")

    def __init__(self, nc, stack, n_dma_sems=48):
        self.nc = nc
        self.streams = {e: [] for e in self.ENGS}
        self.count = {e: 0 for e in self.ENGS}
        self.pending = {e: False for e in self.ENGS}
        self.waited = {e: {} for e in self.ENGS}
        self.sems = {}
        for e in self.ENGS:
            self.sems["E_" + e] = stack.enter_context(nc.semaphore("sem_" + e))
        self.dma_sems = []
        for i in range(n_dma_sems):
            k = "D_%d" % i
            self.sems[k] = stack.enter_context(nc.semaphore("semd_%d" % i))
            self.dma_sems.append(k)
        self.dma_val = {k: 0 for k in self.dma_sems}
        self.dma_rr = 0
        self.dma_rr_sw = 0
        self.n_inst = 0

    def _need(self, eng, tok):
        if tok is None:
            return
        key, val, teng = tok
        if teng == eng:
            if eng == "pe" or not SAME_ENGINE_SYNC or val > self.count[eng]:
                return
        if self.waited[eng].get(key, 0) >= val:
            return
        self.waited[eng][key] = val
        self.streams[eng].append(("wait", key, val))

    def _deps(self, eng, reads, writes):
        for r in reads:
            self._need(eng, r.w)
        for w in writes:
            self._need(eng, w.w)
            for t in w.r:
                self._need(eng, t)

    def _commit(self, tok, reads, writes):
        for r in reads:
            r.r.append(tok)
            if len(r.r) > 48:
                best = {}
                for t in r.r:
                    if t[0] not in best or best[t[0]][1] < t[1]:
                        best[t[0]] = t
                r.r = list(best.values())
        for w in writes:
            w.w = tok
            w.r = []

    def op(self, eng, fn, reads=(), writes=(), signal=True):
        if any(r.excl for r in reads):
            writes = list(writes) + [r for r in reads if r.excl]
            reads = [r for r in reads if not r.excl]
        self._deps(eng, reads, writes)
        if signal:
            self.count[eng] += 1
            val = self.count[eng]
            self.streams[eng].append(("inst", fn, "E_" + eng))
            self.pending[eng] = False
        else:
            val = self.count[eng] + 1
            self.streams[eng].append(("inst", fn, None))
            self.pending[eng] = True
        tok = ("E_" + eng, val, eng)
        self._commit(tok, reads, writes)
        self.n_inst += 1
        return tok

    def dma(self, eng, fn, reads=(), writes=()):
        self._deps(eng, reads, writes)
        half = len(self.dma_sems) // 2
        if eng == "pool":
            key = self.dma_sems[self.dma_rr_sw % half]
            self.dma_rr_sw += 1
        else:
            key = self.dma_sems[half + self.dma_rr % half]
            self.dma_rr += 1
        prev = self.dma_val[key]
        if prev > 0:
            self._need(eng, (key, prev, None))
        self.dma_val[key] = prev + 16
        self.streams[eng].append(("dma", fn, key))
        tok = (key, prev + 16, None)
        self._commit(tok, reads, writes)
        self.n_inst += 1
        return tok

    def barrier(self):
        toks = []
        for e in self.ENGS:
            if self.pending[e]:
                raise RuntimeError("pending unsignalled instrs on " + e)
            if self.count[e] > 0:
                toks.append(("E_" + e, self.count[e], e))
        for k in self.dma_sems:
            if self.dma_val[k] > 0:
                toks.append((k, self.dma_val[k], None))
        for e in self.ENGS:
            for t in toks:
                if t[2] == e:
                    continue
                self._need(e, t)

    def final_wait(self, eng="sp"):
        for k in self.dma_sems:
            if self.dma_val[k] > 0:
                self._need(eng, (k, self.dma_val[k], None))
        for e in self.ENGS:
            if e != eng and self.count[e] > 0:
                self._need(eng, ("E_" + e, self.count[e], e))

    def emit(self):
        nc = self.nc
        sems = self.sems
        streams = self.streams

        def run(engobj, items):
            for it in items:
                if it[0] == "wait":
                    engobj.wait_ge(sems[it[1]], it[2])
                elif it[0] == "inst":
                    ins = it[1](engobj)
                    if it[2] is not None:
                        ins.then_inc(sems[it[2]], 1)
                else:
                    ins = it[1](engobj)
                    ins.then_inc(sems[it[2]], 16)

        with nc.Block() as block:
            @block.tensor
            def _(e):
                run(e, streams["pe"])

            @block.scalar
            def _(e):
                run(e, streams["act"])

            @block.vector
            def _(e):
                run(e, streams["dve"])

            @block.gpsimd
            def _(e):
                run(e, streams["pool"])

            @block.sync
            def _(e):
                run(e, streams["sp"])


class Arena:
    def __init__(self, ap_f32, nwords):
        self.ap = ap_f32
        self.n = nwords
        self.top = 0

    def mark(self):
        return self.top

    def release(self, m):
        self.top = m

    def alloc(self, free_shape, dtype=F32, parts=128):
        nel = 1
        for s in free_shape:
            nel *= s
        esz = 4 if dtype in (F32, I32) else 2
        nw = (nel * esz + 3) // 4
        nw = (nw + 7) // 8 * 8
        if self.top + nw > self.n:
            raise RuntimeError("SBUF arena overflow: need %d words, top %d, cap %d" % (nw, self.top, self.n))
        a = self.ap[0:parts, self.top:self.top + nw]
        self.top += nw
        if dtype != F32:
            a = a.bitcast(dtype)
        a = a[:, 0:nel]
        if len(free_shape) == 2:
            a = a.rearrange("p (a b) -> p a b", a=free_shape[0])
        elif len(free_shape) == 3:
            a = a.rearrange("p (a b c) -> p a b c", a=free_shape[0], b=free_shape[1])
        return a


Q_PAIRS = [(0, 3), (1, 4), (2, 5), (6, 9), (7, 10), (8, 11)]


def build(NL, NSEQ, C, stage="full"):
    T = NSEQ * SEQ
    NT = T // 128
    NG = C // 512
    nc = bass.Bass("TRN2", target_bir_lowering=False)

    def din(name, shape, dt=F32):
        return nc.dram_tensor(name, list(shape), dt, kind="ExternalInput").ap()

    x_in = din("x_in", [T, D])
    cT = din("cT", [D, NSEQ])
    norm1_g = din("norm1_g", [NL, D])
    ada_w = din("ada_w", [NL, D, 6 * D])
    ada_b = din("ada_b", [NL, 6 * D])
    w_in = din("w_in", [NL, D, INW])
    b_in = din("b_in", [NL, INW])
    w_fmix = din("w_fmix", [NL, 4, 64, 64])
    sinks = din("sinks", [NL, 12])
    w_o = din("w_o", [NL, D, D])
    b_o = din("b_o", [NL, D])
    norm2_g = din("norm2_g", [NL, D])
    w_router = din("w_router", [NL, D, NE])
    b_router = din("b_router", [NL, NE])
    w_gu = din("w_gu", [NL, NE, D, 2 * DFF])
    b_gu = din("b_gu", [NL, NE, 2 * DFF])
    w_down = din("w_down", [NL, NE, DFF, D])
    b_down = din("b_down", [NL, NE, D])
    final_g = din("final_g", [1, D])
    dftc = din("dftc", [TPS, 128, TPS * 128], BF16)
    dfts = din("dfts", [TPS, 128, TPS * 128], BF16)
    ccb = din("ccb", [128, 128])
    scb = din("scb", [128, 128])
    rope_c = din("rope_c", [SEQ, 8])
    rope_s = din("rope_s", [SEQ, 8])
    k_ident = din("k_ident", [128, 128])
    k_lstrict = din("k_lstrict", [128, 128])
    k_maska = din("k_maska", [128, 128])
    k_maskb = din("k_maskb", [128, 128])
    k_ebase = din("k_ebase", [128, NE])
    k_trash = din("k_trash", [128, 1])

    y = nc.dram_tensor("y", [T, D], F32, kind="ExternalOutput").ap()
    dbg = nc.dram_tensor("dbg", [128, NL * NE], F32, kind="ExternalOutput").ap()
    xres = nc.dram_tensor("xres", [T, D], F32, kind="Internal").ap()
    Xs = nc.dram_tensor("Xs", [NE * C + 128, D], BF16, kind="Internal").ap()
    Yd = nc.dram_tensor("Yd", [NE * C, D], BF16, kind="Internal").ap()
    modscr = nc.dram_tensor("modscr", [NSEQ, 7, D], F32, kind="Internal").ap()

    with contextlib.ExitStack() as st:
        S = Sched(nc, st)
        ARENA_WORDS = 53120 - 2 * NT * 4 - 16
        arena_t = st.enter_context(nc.sbuf_tensor("arena", [128, ARENA_WORDS], F32))
        AR = Arena(arena_t, ARENA_WORDS)
        ps = [st.enter_context(nc.psum_tensor("psb%d" % i, [128, 512], F32)) for i in range(8)]
        psr = [Res("ps%d" % i, excl=True) for i in range(8)]

        def ps_bf(i):
            return ps[i][:].bitcast(BF16)

        ident_f = AR.alloc([128])
        ident_b = AR.alloc([128], BF16)
        lstrict_b = AR.alloc([128], BF16)
        ones_b = AR.alloc([128], BF16)
        maska_b = AR.alloc([128], BF16)
        maskb_b = AR.alloc([128], BF16)
        ebase = AR.alloc([NE])
        trashc = AR.alloc([1])
        run_cnt = AR.alloc([NE])
        dbg_sb = AR.alloc([NL * NE])
        r_dbg = Res()
        ropec = AR.alloc([TPS, 8])
        ropes = AR.alloc([TPS, 8])
        tmpc = AR.alloc([128])
        slot_sc_t = st.enter_context(nc.sbuf_tensor("slot_sc", [128, NT * 4], I32))
        slot_g_t = st.enter_context(nc.sbuf_tensor("slot_g", [128, NT * 4], I32))
        slot_sc_all = slot_sc_t[:, :].rearrange("p (t k) -> p t k", k=4)
        slot_g_all = slot_g_t[:, :].rearrange("p (t k) -> p t k", k=4)
        gate_all = AR.alloc([NT, 4])
        siluT = AR.alloc([8, NSEQ])
        siluT_r = Res()
        r_const = Res("const")
        r_run = Res("run")
        r_slots = Res("slots")

        def ld(dst, src, res, eng="sp"):
            S.dma(eng, lambda e: e.dma_start(out=dst, in_=src), writes=[res])

        ld(ident_f, k_ident, r_const)
        S.op("dve", lambda e: e.tensor_copy(out=ident_b, in_=ident_f), reads=[r_const], writes=[r_const])
        rr = Res()
        for (dst, src) in ((lstrict_b, k_lstrict), (maska_b, k_maska), (maskb_b, k_maskb)):
            ld(tmpc, src, rr)
            S.op("dve", lambda e, dst=dst: e.tensor_copy(out=dst, in_=tmpc), reads=[rr], writes=[r_const, rr])
        S.op("pool", lambda e: e.memset(ones_b, 1.0), writes=[r_const])
        ld(ebase, k_ebase, r_const)
        ld(trashc, k_trash, r_const)
        ld(ropec, rope_c.rearrange("(t p) i -> p t i", p=128), r_const)
        ld(ropes, rope_s.rearrange("(t p) i -> p t i", p=128), r_const)
        S.dma("sp", lambda e: e.dma_start(out=siluT, in_=cT.rearrange("(c p) s -> p c s", p=128),
                                          allow_slow_non_contiguous=True), writes=[siluT_r])
        S.op("act", lambda e: e.activation(out=siluT, in_=siluT, func=AF.Silu), reads=[siluT_r], writes=[siluT_r])

        w_in_sb = AR.alloc([8, INW], BF16)
        w_o_sb = AR.alloc([8, D], BF16)
        wr_sb = AR.alloc([8, NE], BF16)
        brow = AR.alloc([INW + NE], BF16, parts=1)
        ablk = AR.alloc([2, 128], BF16)
        nbblk = AR.alloc([2, 128], BF16)
        bguT = AR.alloc([8, NE])
        expsink = AR.alloc([12])
        r_win, r_wo, r_wr, r_brow, r_ablk, r_bgu, r_sink = (Res() for _ in range(7))

        phase_mark = AR.mark()
        if stage not in ("A", "A0", "A1", "A3"):
            zt = AR.alloc([4, D], BF16)
            r_zt = Res()
            S.op("pool", lambda e: e.memset(zt, 0.0), writes=[r_zt])
            for r0 in range(0, NE * C, 512):
                S.dma("sp", lambda e, r0=r0: e.dma_start(out=Xs[r0:r0 + 512, :].rearrange("(t p) d -> p t d", p=128),
                                                        in_=zt), reads=[r_zt])
            S.dma("sp", lambda e: e.dma_start(out=Xs[NE * C:NE * C + 128, :], in_=zt[:, 0, :]), reads=[r_zt])
            S.barrier()
            AR.release(phase_mark)

        def xsrc(l):
            return x_in if l == 0 else y

        for l in range(NL):
          try:
              last_layer = (l == NL - 1)
              S.dma("pool", lambda e, l=l: e.dma_start(out=w_in_sb, in_=w_in[l].rearrange("(c p) f -> p c f", p=128)),
                    writes=[r_win])
              S.dma("pool", lambda e, l=l: e.dma_start(out=w_o_sb, in_=w_o[l].rearrange("(c p) f -> p c f", p=128)),
                    writes=[r_wo])
              S.dma("pool", lambda e, l=l: e.dma_start(out=wr_sb, in_=w_router[l].rearrange("(c p) f -> p c f", p=128)),
                    writes=[r_wr])
              S.dma("pool", lambda e, l=l: e.dma_start(out=brow[:, 0:INW], in_=b_in[l:l + 1, :]), writes=[r_brow])
              S.dma("pool", lambda e, l=l: e.dma_start(out=brow[:, INW:INW + NE], in_=b_router[l:l + 1, :]),
                    writes=[r_brow])

              m0 = AR.mark()
              adaw = [AR.alloc([8, 512]) for _ in range(2)]
              r_adaw = [Res(), Res()]
              modblk = [AR.alloc([512], parts=NSEQ) for _ in range(2)]
              r_modblk = [Res(), Res()]
              gainb = AR.alloc([3, D], parts=NSEQ)
              adab = AR.alloc([6 * D], parts=NSEQ)
              r_gain = Res()
              g1keep = AR.alloc([D], parts=NSEQ)
              r_g1keep = Res()
              for i, src in enumerate((norm1_g, norm2_g, b_o)):
                  S.dma("sp", lambda e, i=i, src=src, l=l: e.dma_start(
                      out=gainb[:, i, :], in_=src[l:l + 1, :].partition_broadcast(NSEQ)), writes=[r_gain])
              S.dma("sp", lambda e, l=l: e.dma_start(out=adab, in_=ada_b[l:l + 1, :].partition_broadcast(NSEQ)),
                    writes=[r_gain])
              for cb in range(12):
                  b = cb % 2
                  m = cb // 2
                  hcol = (cb % 2) * 512
                  S.dma("sp", lambda e, l=l, cb=cb, b=b: e.dma_start(
                      out=adaw[b], in_=ada_w[l, :, cb * 512:(cb + 1) * 512].rearrange("(c p) f -> p c f", p=128)),
                      writes=[r_adaw[b]])
                  for kc in range(8):
                      S.op("pe", lambda e, kc=kc, b=b: e.matmul(ps[0][0:NSEQ, :], lhsT=siluT[:, kc, :], rhs=adaw[b][:, kc, :],
                                                               start=(kc == 0), stop=(kc == 7)),
                           reads=[siluT_r, r_adaw[b]], writes=[psr[0]], signal=(kc == 7))
                  mb = modblk[b]
                  S.op("dve", lambda e, mb=mb, cb=cb: e.tensor_tensor(out=mb, in0=ps[0][0:NSEQ, :],
                                                                       in1=adab[:, cb * 512:(cb + 1) * 512], op=ALU.add),
                       reads=[psr[0], r_gain], writes=[r_modblk[b]])
                  if m in (1, 4):
                      gi = 0 if m == 1 else 1
                      S.op("dve", lambda e, mb=mb, gi=gi, hcol=hcol: e.scalar_tensor_tensor(
                          out=mb, in0=mb, scalar=1.0, in1=gainb[:, gi, hcol:hcol + 512], op0=ALU.add, op1=ALU.mult),
                          reads=[r_gain], writes=[r_modblk[b]])
                  dslot = {0: 1, 1: 0, 2: 2, 3: 5, 4: 4, 5: 6}[m]
                  S.dma("sp", lambda e, mb=mb, dslot=dslot, hcol=hcol: e.dma_start(
                      out=modscr[:, dslot, hcol:hcol + 512], in_=mb), reads=[r_modblk[b]])
                  if m == 2:
                      S.op("dve", lambda e, mb=mb, hcol=hcol: e.tensor_tensor(
                          out=g1keep[:, hcol:hcol + 512], in0=mb, in1=gainb[:, 2, hcol:hcol + 512], op=ALU.mult),
                          reads=[r_modblk[b], r_gain], writes=[r_g1keep])
              S.dma("sp", lambda e: e.dma_start(out=modscr[:, 3, :], in_=g1keep), reads=[r_g1keep])

              ccs = AR.alloc([2, 128])
              wblk = AR.alloc([2, 128])
              r_ccs, r_wblk = Res(), Res()
              ld(ccs[:, 0, :], ccb, r_ccs)
              ld(ccs[:, 1, :], scb, r_ccs)
              S.op("pool", lambda e: e.memset(wblk, 0.0), writes=[r_wblk])
              for cc in range(2):
                  for gg in range(2):
                      S.dma("sp", lambda e, l=l, cc=cc, gg=gg: e.dma_start(
                          out=wblk[gg * 64:(gg + 1) * 64, cc, gg * 64:(gg + 1) * 64], in_=w_fmix[l, 2 * cc + gg]),
                          writes=[r_wblk])
              for cc in range(2):
                  for t in range(2):
                      S.op("pe", lambda e, cc=cc, t=t: e.matmul(ps[1][:, t * 256 + cc * 128:t * 256 + (cc + 1) * 128],
                                                               lhsT=ccs[:, t, :], rhs=wblk[:, cc, :], start=True, stop=True),
                           reads=[r_ccs, r_wblk], writes=[psr[1]])
              S.op("act", lambda e: e.activation(out=ablk, in_=ps[1][:, 0:256].rearrange("p (a b) -> p a b", a=2),
                                                 func=AF.Identity), reads=[psr[1]], writes=[r_ablk])
              S.op("act", lambda e: e.activation(out=nbblk, in_=ps[1][:, 256:512].rearrange("p (a b) -> p a b", a=2),
                                                 func=AF.Identity, scale=-1.0), reads=[psr[1]], writes=[r_ablk])
              bgr = AR.alloc([2 * DFF], parts=NE)
              r_bgr = Res()
              ld(bgr, b_gu[l], r_bgr)
              for c8 in range(8):
                  S.op("pe", lambda e, c8=c8: e.transpose(out=ps[2][:, c8 * NE:(c8 + 1) * NE],
                                                          in_=bgr[:, c8 * 128:(c8 + 1) * 128], identity=ident_f[0:NE, 0:NE]),
                       reads=[r_bgr, r_const], writes=[psr[2]], signal=(c8 == 7))
              S.op("act", lambda e: e.activation(out=bguT, in_=ps[2][:, 0:8 * NE].rearrange("p (a b) -> p a b", a=8),
                                                 func=AF.Identity), reads=[psr[2]], writes=[r_bgu])
              S.dma("sp", lambda e, l=l: e.dma_start(out=expsink, in_=sinks[l:l + 1, :].partition_broadcast(128)),
                    writes=[r_sink])
              S.op("act", lambda e: e.activation(out=expsink, in_=expsink, func=AF.Exp), reads=[r_sink], writes=[r_sink])
              S.op("pool", lambda e: e.memset(run_cnt, 0.0), writes=[r_run])
              S.barrier()
              AR.release(m0)
              if stage == 'A0':
                  raise _Stop()

              mA = AR.mark()
              bcf = AR.alloc([7, D])
              r_bc = Res()
              xt = [AR.alloc([D]) for _ in range(2)]
              r_xt = [Res(), Res()]
              tmpf = AR.alloc([D])
              r_tmpf = Res()
              sq = tmpf
              r_sq = r_tmpf
              hb = [AR.alloc([D], BF16) for _ in range(2)]
              r_hb = [Res(), Res()]
              hT = [AR.alloc([8, 128], BF16) for _ in range(2)]
              r_hT = [Res(), Res()]
              stat = AR.alloc([16])
              r_stat = Res()
              f_tm = AR.alloc([TPS, 256], BF16)
              r_f = Res()
              v_aug = AR.alloc([TPS, 4, 66], BF16)
              r_v = Res()
              qk_tm = [AR.alloc([1024], BF16) for _ in range(2)]
              r_qk = [Res(), Res()]
              ropet = AR.alloc([4, 16, 8])
              r_ropet = Res()
              QT = AR.alloc([6, SEQ], BF16)
              KT = AR.alloc([2, SEQ], BF16)
              r_QT, r_KT = Res(), Res()
              dftb = [[AR.alloc([TPS, 128], BF16) for _ in range(2)] for _ in range(2)]
              r_dft = [Res(), Res()]
              ysb = [AR.alloc([2, 2, 128], BF16) for _ in range(2)]
              r_ysb = [Res(), Res()]
              foT = AR.alloc([2, SEQ], BF16)
              r_foT = Res()
              PT = [AR.alloc([3, 128], BF16) for _ in range(4)]
              r_PT = [Res() for _ in range(4)]
              den = AR.alloc([12])
              r_den = Res()
              attn_tm = [AR.alloc([12, 64], BF16) for _ in range(2)]
              r_attn = [Res(), Res()]
              attnT = [AR.alloc([6, 128], BF16) for _ in range(2)]
              r_attnT = [Res(), Res()]
              xb = AR.alloc([D])
              r_xb = Res()
              xmid_ = AR.alloc([D])
              xmid = [xmid_, xmid_]
              r_xmid_ = Res()
              r_xmid = [r_xmid_, r_xmid_]
              h2 = [AR.alloc([D], BF16) for _ in range(2)]
              r_h2 = [Res(), Res()]
              h2T = AR.alloc([8, 128], BF16)
              r_h2T = Res()
              rt_lg = AR.alloc([NE])
              rt_t8 = AR.alloc([8])
              rt_mask = AR.alloc([NE])
              rt_maskb = AR.alloc([NE], BF16)
              rt_e = AR.alloc([NE])
              rt_G = AR.alloc([NE])
              rt_cnt = AR.alloc([NE])
              rt_ms = AR.alloc([NE])
              rt_s8 = AR.alloc([8])
              rt_small = AR.alloc([16])
              rt_junk = AR.alloc([NE])
              r_rt = Res()
              S.op("pool", lambda e: e.memset(v_aug, 1.0), writes=[r_v])

              def rms_mod(xtile, r_x, a_idx, s_idx, out_b, r_out):
                  S.op("act", lambda e: e.activation(out=sq, in_=xtile, func=AF.Square, accum_out=stat[:, 0:1]),
                       reads=[r_x], writes=[r_sq, r_stat])
                  S.op("act", lambda e: e.activation(out=stat[:, 1:2], in_=stat[:, 0:1], func=AF.Sqrt,
                                                     scale=1.0 / D, bias=EPS), reads=[r_stat], writes=[r_stat])
                  S.op("dve", lambda e: e.reciprocal(out=stat[:, 2:3], in_=stat[:, 1:2]), reads=[r_stat], writes=[r_stat])
                  S.op("dve", lambda e: e.scalar_tensor_tensor(out=tmpf, in0=xtile, scalar=stat[:, 2:3],
                                                               in1=bcf[:, a_idx, :], op0=ALU.mult, op1=ALU.mult),
                       reads=[r_x, r_stat, r_bc], writes=[r_tmpf])
                  S.op("pool", lambda e: e.tensor_tensor(out=out_b, in0=tmpf, in1=bcf[:, s_idx, :], op=ALU.add),
                       reads=[r_tmpf, r_bc], writes=[r_out])

              def transpose8(src_b, r_src, bank, dst, r_dst, evac_eng):
                  pv = ps_bf(bank).rearrange("p (c t) -> p c t", c=8)
                  for c in range(8):
                      S.op("pe", lambda e, c=c: e.transpose(out=pv[:, c, :], in_=src_b[:, c * 128:(c + 1) * 128],
                                                            identity=ident_b), reads=[r_src, r_const],
                           writes=[psr[bank]], signal=(c == 7))
                  if evac_eng == "act":
                      S.op("act", lambda e: e.activation(out=dst, in_=pv, func=AF.Identity),
                           reads=[psr[bank]], writes=[r_dst])
                  else:
                      S.op("dve", lambda e: e.tensor_copy(out=dst, in_=pv), reads=[psr[bank]], writes=[r_dst])

              for s in range(NSEQ):
                  S.dma("sp", lambda e, s=s: e.dma_start(
                      out=bcf, in_=modscr[s:s + 1].rearrange("o a d -> o (a d)").partition_broadcast(128)),
                      writes=[r_bc])
                  for tt in range(TPS):
                      gt = s * TPS + tt
                      b = tt % 2
                      S.dma("sp", lambda e, gt=gt, b=b, l=l: e.dma_start(out=xt[b], in_=xsrc(l)[gt * 128:(gt + 1) * 128, :]),
                            writes=[r_xt[b]])
                      rms_mod(xt[b], r_xt[b], 0, 1, hb[b], r_hb[b])
                      if stage == 'A1a':
                          raise _Stop()
                      transpose8(hb[b], r_hb[b], 7, hT[b], r_hT[b], "act")
                      if stage == 'A1b':
                          raise _Stop()
                      for cb in range(3):
                          for kc in range(8):
                              S.op("pe", lambda e, cb=cb, kc=kc, b=b: e.matmul(
                                  ps[cb][:], lhsT=hT[b][:, kc, :], rhs=w_in_sb[:, kc, cb * 512:(cb + 1) * 512],
                                  start=(kc == 0), stop=False), reads=[r_hT[b], r_win], writes=[psr[cb]], signal=False)
                          S.op("pe", lambda e, cb=cb: e.matmul(
                              ps[cb][:], lhsT=ones_b[0:1, :], rhs=brow[0:1, cb * 512:(cb + 1) * 512],
                              start=False, stop=True), reads=[r_const, r_brow], writes=[psr[cb]])
                      if stage == 'A1c':
                          raise _Stop()
                      S.op("act", lambda e, tt=tt: e.activation(out=f_tm[:, tt, :], in_=ps[0][:, 0:256], func=AF.Identity),
                           reads=[psr[0]], writes=[r_f])
                      qb = qk_tm[b]
                      zst = xt[b]
                      rz = r_xt[b]
                      S.op("act", lambda e, zst=zst: e.activation(out=zst[:, 0:256], in_=ps[0][:, 256:512], func=AF.Identity),
                           reads=[psr[0]], writes=[rz])
                      S.op("act", lambda e, zst=zst: e.activation(out=zst[:, 256:768], in_=ps[1][:], func=AF.Identity),
                           reads=[psr[1]], writes=[rz])
                      S.op("act", lambda e, zst=zst: e.activation(out=zst[:, 768:1024], in_=ps[2][:, 0:256], func=AF.Identity),
                           reads=[psr[2]], writes=[rz])
                      if stage == 'A1c1':
                          raise _Stop()
                      S.op("act", lambda e, tt=tt: e.activation(
                          out=v_aug[:, tt, :, 0:64], in_=ps[2][:, 256:512].rearrange("p (h d) -> p h d", h=4),
                          func=AF.Identity), reads=[psr[2]], writes=[r_v])
                      if stage == 'A1c2':
                          raise _Stop()
                      cosb = ropec[:, tt, :].unsqueeze(1).to_broadcast([128, 16, 8])
                      sinb = ropes[:, tt, :].unsqueeze(1).to_broadcast([128, 16, 8])
                      zv = zst.rearrange("p (h d) -> p h d", h=16)
                      x1 = zv[:, :, 0:8]
                      x2 = zv[:, :, 8:16]
                      t0, t1, t2, t3 = (ropet[:, i, :, :] for i in range(4))
                      S.op("dve", lambda e, t0=t0, x1=x1, cosb=cosb: e.tensor_tensor(out=t0, in0=x1, in1=cosb, op=ALU.mult),
                           reads=[rz, r_const], writes=[r_ropet])
                      S.op("dve", lambda e, t1=t1, x2=x2, sinb=sinb: e.tensor_tensor(out=t1, in0=x2, in1=sinb, op=ALU.mult),
                           reads=[rz, r_const], writes=[r_ropet])
                      S.op("dve", lambda e, t2=t2, x2=x2, cosb=cosb: e.tensor_tensor(out=t2, in0=x2, in1=cosb, op=ALU.mult),
                           reads=[rz, r_const], writes=[r_ropet])
                      S.op("dve", lambda e, t3=t3, x1=x1, sinb=sinb: e.tensor_tensor(out=t3, in0=x1, in1=sinb, op=ALU.mult),
                           reads=[rz, r_const], writes=[r_ropet])
                      S.op("dve", lambda e, x1=x1, t0=t0, t1=t1: e.tensor_tensor(out=x1, in0=t0, in1=t1, op=ALU.subtract),
                           reads=[r_ropet], writes=[rz])
                      S.op("dve", lambda e, x2=x2, t2=t2, t3=t3: e.tensor_tensor(out=x2, in0=t2, in1=t3, op=ALU.add),
                           reads=[r_ropet], writes=[rz])
                      if stage == 'A1d':
                          raise _Stop()
                      qdst = qb[:, 0:768].rearrange("p (h d) -> p h d", h=12)
                      for half6 in range(2):
                          src = zv[:, half6 * 6:(half6 + 1) * 6, :].rearrange("p (a b) d -> p a b d", a=2)
                          dst = qdst[:, half6 * 6:(half6 + 1) * 6, :].rearrange("p (b a) d -> p a b d", a=2)
                          S.op("pool", lambda e, src=src, dst=dst: e.tensor_copy(out=dst, in_=src), reads=[rz],
                               writes=[r_qk[b]])
                      S.op("act", lambda e, zst=zst, qb=qb: e.activation(out=qb[:, 768:1024], in_=zst[:, 768:1024],
                                                                         func=AF.Identity), reads=[rz], writes=[r_qk[b]])
                      pq = ps_bf(3).rearrange("p (c t) -> p c t", c=8)
                      for c in range(8):
                          S.op("pe", lambda e, c=c, qb=qb: e.transpose(out=pq[:, c, :], in_=qb[:, c * 128:(c + 1) * 128],
                                                                       identity=ident_b),
                               reads=[r_qk[b], r_const], writes=[psr[3]], signal=(c == 7))
                      S.op("act", lambda e, tt=tt: e.activation(out=QT[:, :, tt * 128:(tt + 1) * 128], in_=pq[:, 0:6, :],
                                                                func=AF.Identity), reads=[psr[3]], writes=[r_QT])
                      S.op("act", lambda e, tt=tt: e.activation(out=KT[:, :, tt * 128:(tt + 1) * 128], in_=pq[:, 6:8, :],
                                                                func=AF.Identity), reads=[psr[3]], writes=[r_KT])

                  if stage == 'A1':
                      raise _Stop()
                  for kb in range(SEQ // 128):
                      b = kb % 2
                      S.dma("sp", lambda e, kb=kb, b=b: e.dma_start(
                          out=dftb[b][0], in_=dftc[kb].rearrange("p (t k) -> p t k", t=TPS)),
                          writes=[r_dft[b]])
                      S.dma("sp", lambda e, kb=kb, b=b: e.dma_start(
                          out=dftb[b][1], in_=dfts[kb].rearrange("p (t k) -> p t k", t=TPS)),
                          writes=[r_dft[b]])
                      for t in range(2):
                          for cc in range(2):
                              for stile in range(TPS):
                                  S.op("pe", lambda e, t=t, cc=cc, stile=stile, b=b: e.matmul(
                                      ps[4][:, (t * 2 + cc) * 128:(t * 2 + cc + 1) * 128],
                                      lhsT=f_tm[:, stile, cc * 128:(cc + 1) * 128], rhs=dftb[b][t][:, stile, :],
                                      start=(stile == 0), stop=(stile == TPS - 1)),
                                      reads=[r_f, r_dft[b]], writes=[psr[4]], signal=(stile == TPS - 1))
                      S.op("act", lambda e, b=b: e.activation(
                          out=ysb[b], in_=ps[4][:].rearrange("p (a c k) -> p a c k", a=2, c=2), func=AF.Identity),
                          reads=[psr[4]], writes=[r_ysb[b]])
                      for cc in range(2):
                          S.op("pe", lambda e, cc=cc, b=b: e.matmul(ps[5][:, cc * 128:(cc + 1) * 128], lhsT=ablk[:, cc, :],
                                                                   rhs=ysb[b][:, 0, cc, :], start=True, stop=False),
                               reads=[r_ablk, r_ysb[b]], writes=[psr[5]], signal=False)
                          S.op("pe", lambda e, cc=cc, b=b: e.matmul(ps[5][:, cc * 128:(cc + 1) * 128], lhsT=nbblk[:, cc, :],
                                                                   rhs=ysb[b][:, 1, cc, :], start=False, stop=True),
                               reads=[r_ablk, r_ysb[b]], writes=[psr[5]])
                      S.op("dve", lambda e, kb=kb: e.tensor_copy(
                          out=foT[:, :, kb * 128:(kb + 1) * 128], in_=ps[5][:, 0:256].rearrange("p (c k) -> p c k", c=2)),
                          reads=[psr[5]], writes=[r_foT])

                  if stage == 'A3':
                      raise _Stop()
                  for n in range(TPS):
                      gt = s * TPS + n
                      ab = n % 2
                      js = [j for j in (n - 1, n, n + 1) if 0 <= j < TPS]
                      for g in range(4):
                          off = (g % 2) * 64
                          kc_ = g // 2
                          pts = []
                          for ji, j in enumerate(js):
                              sbank = 3 + (ji % 2)
                              pt_i = (g * 3 + ji) % 4
                              for i in range(3):
                                  S.op("pe", lambda e, i=i, j=j, off=off, kc_=kc_, sbank=sbank, n=n: e.matmul(
                                      ps[sbank][:, i * 128:(i + 1) * 128],
                                      lhsT=KT[off:off + 64, kc_, j * 128:(j + 1) * 128],
                                      rhs=QT[off:off + 64, kc_ * 3 + i, n * 128:(n + 1) * 128], start=True, stop=True),
                                      reads=[r_KT, r_QT], writes=[psr[sbank]], signal=(i == 2))
                              ptile = PT[pt_i]
                              S.op("act", lambda e, ptile=ptile, sbank=sbank: e.activation(
                                  out=ptile, in_=ps[sbank][:, 0:384].rearrange("p (i q) -> p i q", i=3), func=AF.Exp,
                                  scale=0.125), reads=[psr[sbank]], writes=[r_PT[pt_i]])
                              if j != n:
                                  mk = maska_b if j < n else maskb_b
                                  S.op("dve", lambda e, ptile=ptile, mk=mk: e.tensor_tensor(
                                      out=ptile, in0=ptile, in1=mk.unsqueeze(1).to_broadcast([128, 3, 128]), op=ALU.mult),
                                      reads=[r_const], writes=[r_PT[pt_i]])
                              pts.append((j, ptile, r_PT[pt_i]))
                          for i in range(3):
                              h = 3 * g + i
                              obank = 5 + h // 6
                              ocol = (h % 6) * 65
                              for ji, (j, ptile, rpt) in enumerate(pts):
                                  S.op("pe", lambda e, i=i, j=j, g=g, ptile=ptile, obank=obank, ocol=ocol, ji=ji,
                                       nj=len(pts): e.matmul(
                                      ps[obank][:, ocol:ocol + 65], lhsT=ptile[:, i, :], rhs=v_aug[:, j, g, 0:65],
                                      start=(ji == 0), stop=(ji == nj - 1)),
                                      reads=[rpt, r_v], writes=[psr[obank]], signal=(ji == len(pts) - 1))
                      for hb2 in range(2):
                          ov = ps[5 + hb2][:, 0:390].rearrange("p (h d) -> p h d", h=6)
                          S.op("dve", lambda e, ov=ov, hb2=hb2: e.tensor_tensor(
                              out=den[:, hb2 * 6:(hb2 + 1) * 6].unsqueeze(2), in0=ov[:, :, 64:65],
                              in1=expsink[:, hb2 * 6:(hb2 + 1) * 6].unsqueeze(2), op=ALU.add),
                              reads=[psr[5 + hb2], r_sink], writes=[r_den])
                          S.op("dve", lambda e, hb2=hb2: e.reciprocal(out=den[:, hb2 * 6:(hb2 + 1) * 6],
                                                                      in_=den[:, hb2 * 6:(hb2 + 1) * 6]),
                               reads=[r_den], writes=[r_den])
                          S.op("dve", lambda e, ov=ov, hb2=hb2, ab=ab: e.tensor_tensor(
                              out=attn_tm[ab][:, hb2 * 6:(hb2 + 1) * 6, :], in0=ov[:, :, 0:64],
                              in1=den[:, hb2 * 6:(hb2 + 1) * 6].unsqueeze(2).to_broadcast([128, 6, 64]), op=ALU.mult),
                              reads=[psr[5 + hb2], r_den], writes=[r_attn[ab]])
                      av = attn_tm[ab].rearrange("p h d -> p (h d)")
                      pa = ps_bf(7).rearrange("p (c t) -> p c t", c=8)
                      for c in range(6):
                          S.op("pe", lambda e, c=c, av=av: e.transpose(out=pa[:, c, :], in_=av[:, c * 128:(c + 1) * 128],
                                                                       identity=ident_b),
                               reads=[r_attn[ab], r_const], writes=[psr[7]], signal=(c == 5))
                      S.op("act", lambda e, ab=ab: e.activation(out=attnT[ab], in_=pa[:, 0:6, :], func=AF.Identity),
                           reads=[psr[7]], writes=[r_attnT[ab]])
                      for half in range(2):
                          for kc in range(8):
                              if kc < 2:
                                  lhs = foT[:, kc, n * 128:(n + 1) * 128]
                                  rd = [r_foT, r_wo]
                              else:
                                  lhs = attnT[ab][:, kc - 2, :]
                                  rd = [r_attnT[ab], r_wo]
                              S.op("pe", lambda e, half=half, kc=kc, lhs=lhs: e.matmul(
                                  ps[half][:], lhsT=lhs, rhs=w_o_sb[:, kc, half * 512:(half + 1) * 512],
                                  start=(kc == 0), stop=(kc == 7)), reads=rd, writes=[psr[half]], signal=(kc == 7))
                      mb_ = n % 2
                      S.dma("sp", lambda e, gt=gt, l=l: e.dma_start(out=xb, in_=xsrc(l)[gt * 128:(gt + 1) * 128, :]),
                            writes=[r_xb])
                      S.op("pool", lambda e: e.tensor_tensor(out=xb, in0=xb, in1=bcf[:, 3, :], op=ALU.add),
                           reads=[r_bc], writes=[r_xb])
                      for half in range(2):
                          S.op("dve", lambda e, half=half: e.tensor_tensor(
                              out=tmpf[:, half * 512:(half + 1) * 512], in0=ps[half][:],
                              in1=bcf[:, 2, half * 512:(half + 1) * 512], op=ALU.mult),
                              reads=[psr[half], r_bc], writes=[r_tmpf])
                      S.op("pool", lambda e, mb_=mb_: e.tensor_tensor(out=xmid[mb_], in0=tmpf, in1=xb, op=ALU.add),
                           reads=[r_tmpf, r_xb], writes=[r_xmid[mb_]])
                      if stage == "A":
                          S.dma("sp", lambda e, gt=gt, mb_=mb_: e.dma_start(out=y[gt * 128:(gt + 1) * 128, :], in_=xmid[mb_]),
                                reads=[r_xmid[mb_]])
                          continue
                      S.dma("sp", lambda e, gt=gt, mb_=mb_: e.dma_start(out=xres[gt * 128:(gt + 1) * 128, :], in_=xmid[mb_]),
                            reads=[r_xmid[mb_]])
                      rms_mod(xmid[mb_], r_xmid[mb_], 4, 5, h2[mb_], r_h2[mb_])
                      transpose8(h2[mb_], r_h2[mb_], 7, h2T, r_h2T, "act")
                      for kc in range(8):
                          S.op("pe", lambda e, kc=kc: e.matmul(ps[2][:, 0:NE], lhsT=h2T[:, kc, :], rhs=wr_sb[:, kc, :],
                                                               start=(kc == 0), stop=False),
                               reads=[r_h2T, r_wr], writes=[psr[2]], signal=False)
                      S.op("pe", lambda e: e.matmul(ps[2][:, 0:NE], lhsT=ones_b[0:1, :], rhs=brow[0:1, INW:INW + NE],
                                                    start=False, stop=True), reads=[r_const, r_brow], writes=[psr[2]])
                      R = [r_rt]
                      S.op("dve", lambda e: e.tensor_copy(out=rt_lg, in_=ps[2][:, 0:NE]), reads=[psr[2]], writes=R)
                      S.op("dve", lambda e: e.max(out=rt_t8, in_=rt_lg), writes=R)
                      S.op("dve", lambda e: e.tensor_scalar(out=rt_mask, in0=rt_lg, scalar1=rt_t8[:, 3:4], scalar2=None,
                                                            op0=ALU.is_ge), writes=R)
                      S.op("dve", lambda e: e.tensor_copy(out=rt_maskb, in_=rt_mask), writes=R)
                      S.op("dve", lambda e: e.tensor_scalar(out=rt_small[:, 0:1], in0=rt_t8[:, 0:1], scalar1=-1.0,
                                                            scalar2=None, op0=ALU.mult), writes=R)
                      S.op("act", lambda e: e.activation(out=rt_e, in_=rt_lg, func=AF.Exp, bias=rt_small[:, 0:1]),
                           reads=R, writes=R)
                      S.op("dve", lambda e: e.scalar_tensor_tensor(out=rt_G, in0=rt_e, scalar=1.0, in1=rt_mask,
                                                                   op0=ALU.mult, op1=ALU.mult,
                                                                   accum_out=rt_small[:, 1:2]), reads=R, writes=R)
                      S.op("dve", lambda e: e.reciprocal(out=rt_small[:, 2:3], in_=rt_small[:, 1:2]), writes=R)
                      S.op("dve", lambda e: e.tensor_scalar(out=rt_G, in0=rt_G, scalar1=rt_small[:, 2:3], scalar2=None,
                                                            op0=ALU.mult), writes=R)
                      S.op("pe", lambda e: e.matmul(ps[2][:, 64:64 + NE], lhsT=lstrict_b, rhs=rt_maskb, start=True, stop=True),
                           reads=[r_rt, r_const], writes=[psr[2]])
                      S.op("pe", lambda e: e.matmul(ps[2][:, 128:128 + NE], lhsT=ones_b, rhs=rt_maskb, start=True, stop=True),
                           reads=[r_rt, r_const], writes=[psr[2]])
                      S.op("dve", lambda e: e.tensor_tensor(out=rt_cnt, in0=ps[2][:, 64:64 + NE], in1=run_cnt, op=ALU.add),
                           reads=[psr[2], r_run], writes=R)
                      S.op("dve", lambda e: e.tensor_tensor(out=run_cnt, in0=ps[2][:, 128:128 + NE], in1=run_cnt, op=ALU.add),
                           reads=[psr[2]], writes=[r_run])
                      S.op("dve", lambda e: e.scalar_tensor_tensor(out=rt_ms, in0=rt_cnt, scalar=float(C), in1=rt_mask,
                                                                   op0=ALU.is_lt, op1=ALU.mult), writes=R)
                      S.op("dve", lambda e: e.tensor_tensor(out=rt_G, in0=rt_G, in1=rt_ms, op=ALU.mult), writes=R)
                      S.op("dve", lambda e: e.scalar_tensor_tensor(out=rt_cnt, in0=rt_cnt, scalar=1.0, in1=ebase,
                                                                   op0=ALU.add, op1=ALU.add), reads=[r_const], writes=R)
                      S.op("dve", lambda e: e.tensor_tensor(out=rt_ms, in0=rt_ms, in1=rt_cnt, op=ALU.mult), writes=R)
                      S.op("dve", lambda e: e.max(out=rt_s8, in_=rt_ms), writes=R)
                      for k in range(4):
                          S.op("dve", lambda e, k=k, gt=gt: e.scalar_tensor_tensor(
                              out=rt_junk, in0=rt_ms, scalar=rt_s8[:, k:k + 1], in1=rt_G, op0=ALU.is_equal, op1=ALU.mult,
                              accum_out=gate_all[:, gt, k:k + 1]), writes=R + [r_slots])
                      S.op("dve", lambda e, gt=gt: e.tensor_scalar(out=slot_g_all[:, gt, :], in0=rt_s8[:, 0:4], scalar1=1.0,
                                                                   scalar2=-1.0, op0=ALU.max, op1=ALU.add),
                           writes=R + [r_slots])
                      S.op("dve", lambda e: e.tensor_scalar(out=rt_small[:, 4:8], in0=rt_s8[:, 0:4], scalar1=0.5,
                                                            scalar2=trashc[:, 0:1], op0=ALU.is_lt, op1=ALU.mult),
                           reads=[r_const], writes=R)
                      S.op("dve", lambda e: e.scalar_tensor_tensor(out=rt_small[:, 8:12], in0=rt_s8[:, 0:4], scalar=-1.0,
                                                                   in1=rt_small[:, 4:8], op0=ALU.add, op1=ALU.add), writes=R)
                      S.op("dve", lambda e, gt=gt: e.tensor_copy(out=slot_sc_all[:, gt, :], in_=rt_small[:, 8:12]),
                           writes=R + [r_slots])
                      for k in range(4):
                          S.dma("pool", lambda e, k=k, gt=gt, mb_=mb_: e.indirect_dma_start(
                              out=Xs, out_offset=bass.IndirectOffsetOnAxis(ap=slot_sc_t[:, gt * 4 + k:gt * 4 + k + 1], axis=0),
                              in_=h2[mb_], in_offset=None),
                              reads=[r_slots, r_h2[mb_]])
              S.op("dve", lambda e, l=l: e.tensor_copy(out=dbg_sb[:, l * NE:(l + 1) * NE], in_=run_cnt),
                   reads=[r_run], writes=[r_dbg])
              S.barrier()
              AR.release(mA)
              if stage == "A":
                  continue

              mB = AR.mark()
              wgu = [AR.alloc([8, 2 * DFF], BF16) for _ in range(2)]
              wdn = [AR.alloc([4, D], BF16) for _ in range(2)]
              bdb = [AR.alloc([D]) for _ in range(2)]
              r_w = [Res(), Res()]
              xs = [AR.alloc([4, D], BF16) for _ in range(2)]
              r_xs = [Res(), Res()]
              XT = AR.alloc([8, 512], BF16)
              r_XT = Res()
              gcb = AR.alloc([512])
              uab = AR.alloc([512])
              sgb = AR.alloc([512])
              ucb = AR.alloc([512])
              tb = AR.alloc([512])
              r_gc, r_ua, r_sg, r_uc, r_tb = (Res() for _ in range(5))
              actT = [AR.alloc([4, 512], BF16) for _ in range(2)]
              r_act = [Res(), Res()]
              ysbuf = [AR.alloc([4, D], BF16) for _ in range(2)]
              r_ys = [Res(), Res()]
              it = 0
              for ex in range(NE):
                  wb = ex % 2
                  S.dma("pool", lambda e, l=l, ex=ex, wb=wb: e.dma_start(
                      out=wgu[wb], in_=w_gu[l, ex].rearrange("(c p) f -> p c f", p=128)), writes=[r_w[wb]])
                  S.dma("pool", lambda e, l=l, ex=ex, wb=wb: e.dma_start(
                      out=wdn[wb], in_=w_down[l, ex].rearrange("(c p) f -> p c f", p=128)), writes=[r_w[wb]])
                  S.dma("sp", lambda e, l=l, ex=ex, wb=wb: e.dma_start(
                      out=bdb[wb], in_=b_down[l, ex:ex + 1, :].partition_broadcast(128)), writes=[r_w[wb]])
                  for g in range(NG):
                      r0 = ex * C + g * 512
                      ib = it % 2
                      it += 1
                      S.dma("sp", lambda e, r0=r0, ib=ib: e.dma_start(
                          out=xs[ib], in_=Xs[r0:r0 + 512, :].rearrange("(t p) d -> p t d", p=128)), writes=[r_xs[ib]])
                      for rt in range(4):
                          bank = 6 + (rt % 2)
                          pv = ps_bf(bank).rearrange("p (c t) -> p c t", c=8)
                          for c in range(8):
                              S.op("pe", lambda e, c=c, rt=rt, ib=ib, pv=pv: e.transpose(
                                  out=pv[:, c, :], in_=xs[ib][:, rt, c * 128:(c + 1) * 128], identity=ident_b),
                                  reads=[r_xs[ib], r_const], writes=[psr[bank]], signal=(c == 7))
                          if rt % 2 == 0:
                              S.op("act", lambda e, rt=rt, pv=pv: e.activation(out=XT[:, :, rt * 128:(rt + 1) * 128], in_=pv,
                                                                               func=AF.Identity),
                                   reads=[psr[bank]], writes=[r_XT])
                          else:
                              S.op("dve", lambda e, rt=rt, pv=pv: e.tensor_copy(out=XT[:, :, rt * 128:(rt + 1) * 128], in_=pv),
                                   reads=[psr[bank]], writes=[r_XT])
                      for pr in range(4):
                          gbank = (pr % 2) * 2
                          ubank = gbank + 1
                          for (fo, bank) in ((pr, gbank), (pr + 4, ubank)):
                              for kc in range(8):
                                  S.op("pe", lambda e, fo=fo, bank=bank, kc=kc, wb=wb: e.matmul(
                                      ps[bank][:], lhsT=wgu[wb][:, kc, fo * 128:(fo + 1) * 128], rhs=XT[:, kc, :],
                                      start=(kc == 0), stop=(kc == 7)), reads=[r_w[wb], r_XT], writes=[psr[bank]],
                                      signal=(kc == 7))
                          S.op("act", lambda e, pr=pr, ex=ex, ubank=ubank: e.activation(
                              out=uab, in_=ps[ubank][:], func=AF.Identity, bias=bguT[:, pr + 4, ex:ex + 1]),
                              reads=[psr[ubank], r_bgu], writes=[r_ua])
                          S.op("dve", lambda e, pr=pr, ex=ex, gbank=gbank: e.tensor_scalar(
                              out=gcb, in0=ps[gbank][:], scalar1=bguT[:, pr, ex:ex + 1], scalar2=7.0, op0=ALU.add,
                              op1=ALU.min), reads=[psr[gbank], r_bgu], writes=[r_gc])
                          S.op("act", lambda e: e.activation(out=sgb, in_=gcb, func=AF.Sigmoid, scale=1.702),
                               reads=[r_gc], writes=[r_sg])
                          S.op("pool", lambda e: e.tensor_scalar(out=ucb, in0=uab, scalar1=7.0, scalar2=-7.0, op0=ALU.min,
                                                                 op1=ALU.max), reads=[r_ua], writes=[r_uc])
                          S.op("dve", lambda e: e.tensor_tensor(out=tb, in0=gcb, in1=sgb, op=ALU.mult),
                               reads=[r_gc, r_sg], writes=[r_tb])
                          S.op("dve", lambda e, pr=pr, ib=ib: e.scalar_tensor_tensor(
                              out=actT[ib][:, pr, :], in0=ucb, scalar=1.0, in1=tb, op0=ALU.add, op1=ALU.mult),
                              reads=[r_uc, r_tb], writes=[r_act[ib]])
                      for rt in range(4):
                          for half in range(2):
                              bank = 4 + half
                              for fc in range(4):
                                  S.op("pe", lambda e, rt=rt, half=half, fc=fc, ib=ib, wb=wb, bank=bank: e.matmul(
                                      ps[bank][:], lhsT=actT[ib][:, fc, rt * 128:(rt + 1) * 128],
                                      rhs=wdn[wb][:, fc, half * 512:(half + 1) * 512], start=(fc == 0), stop=(fc == 3)),
                                      reads=[r_act[ib], r_w[wb]], writes=[psr[bank]], signal=(fc == 3))
                              S.op("dve", lambda e, rt=rt, half=half, ib=ib, wb=wb, bank=bank: e.tensor_tensor(
                                  out=ysbuf[ib][:, rt, half * 512:(half + 1) * 512], in0=ps[bank][:],
                                  in1=bdb[wb][:, half * 512:(half + 1) * 512], op=ALU.add),
                                  reads=[psr[bank], r_w[wb]], writes=[r_ys[ib]])
                      S.dma("sp", lambda e, r0=r0, ib=ib: e.dma_start(
                          out=Yd[r0:r0 + 512, :].rearrange("(t p) d -> p t d", p=128), in_=ysbuf[ib]), reads=[r_ys[ib]])
              S.barrier()
              AR.release(mB)

              mC = AR.mark()
              g2b = AR.alloc([D])
              r_g2 = Res()
              fgb = AR.alloc([D])
              r_fg = Res()
              xm = [AR.alloc([D]) for _ in range(2)]
              r_xm = [Res(), Res()]
              yk = [[AR.alloc([D], BF16) for _ in range(4)] for _ in range(2)]
              r_yk = [Res(), Res()]
              acc = [AR.alloc([D]) for _ in range(2)]
              r_acc = [Res(), Res()]
              outb = [AR.alloc([D]) for _ in range(2)]
              r_out = [Res(), Res()]
              sq2 = AR.alloc([D], BF16)
              r_sq2 = Res()
              st2 = AR.alloc([8])
              r_st2 = Res()
              if last_layer:
                  S.dma("sp", lambda e: e.dma_start(out=fgb, in_=final_g.partition_broadcast(128)), writes=[r_fg])
              for gt in range(NT):
                  s = gt // TPS
                  b = gt % 2
                  if gt % TPS == 0:
                      S.dma("sp", lambda e, s=s: e.dma_start(out=g2b, in_=modscr[s, 6:7, :].partition_broadcast(128)),
                            writes=[r_g2])
                  S.dma("sp", lambda e, gt=gt, b=b: e.dma_start(out=xm[b], in_=xres[gt * 128:(gt + 1) * 128, :]),
                        writes=[r_xm[b]])
                  for k in range(4):
                      S.dma("pool", lambda e, gt=gt, b=b, k=k: e.indirect_dma_start(
                          out=yk[b][k], out_offset=None, in_=Yd,
                          in_offset=bass.IndirectOffsetOnAxis(ap=slot_g_t[:, gt * 4 + k:gt * 4 + k + 1], axis=0)),
                          reads=[r_slots], writes=[r_yk[b]])
                  S.op("dve", lambda e, gt=gt, b=b: e.tensor_scalar(out=acc[b], in0=yk[b][0], scalar1=gate_all[:, gt, 0:1],
                                                                    scalar2=None, op0=ALU.mult),
                       reads=[r_yk[b], r_slots], writes=[r_acc[b]])
                  for k in range(1, 4):
                      S.op("dve", lambda e, gt=gt, b=b, k=k: e.scalar_tensor_tensor(
                          out=acc[b], in0=yk[b][k], scalar=gate_all[:, gt, k:k + 1], in1=acc[b], op0=ALU.mult, op1=ALU.add),
                          reads=[r_yk[b], r_slots], writes=[r_acc[b]])
                  S.op("pool", lambda e, b=b: e.tensor_tensor(out=acc[b], in0=acc[b], in1=g2b, op=ALU.mult),
                       reads=[r_g2], writes=[r_acc[b]])
                  S.op("pool", lambda e, b=b: e.tensor_tensor(out=outb[b], in0=acc[b], in1=xm[b], op=ALU.add),
                       reads=[r_acc[b], r_xm[b]], writes=[r_out[b]])
                  if last_layer:
                      S.op("act", lambda e, b=b: e.activation(out=sq2, in_=outb[b], func=AF.Square, accum_out=st2[:, 0:1]),
                           reads=[r_out[b]], writes=[r_sq2, r_st2])
                      S.op("act", lambda e: e.activation(out=st2[:, 1:2], in_=st2[:, 0:1], func=AF.Sqrt, scale=1.0 / D,
                                                         bias=EPS), reads=[r_st2], writes=[r_st2])
                      S.op("dve", lambda e: e.reciprocal(out=st2[:, 2:3], in_=st2[:, 1:2]), reads=[r_st2], writes=[r_st2])
                      S.op("dve", lambda e, b=b: e.scalar_tensor_tensor(out=outb[b], in0=outb[b], scalar=st2[:, 2:3],
                                                                        in1=fgb, op0=ALU.mult, op1=ALU.mult),
                           reads=[r_st2, r_fg], writes=[r_out[b]])
                  S.dma("sp", lambda e, gt=gt, b=b: e.dma_start(out=y[gt * 128:(gt + 1) * 128, :], in_=outb[b]),
                        reads=[r_out[b]])
              S.barrier()
              AR.release(mC)

          except _Stop:
            S.barrier()
            break
        if stage == "full":
            S.dma("sp", lambda e: e.dma_start(out=dbg, in_=dbg_sb), reads=[r_dbg])
        S.final_wait("sp")
        S.emit()
        print("instructions:", S.n_inst, flush=True)
    return nc


def make_consts():
    bf = ml_dtypes.bfloat16
    s = np.arange(SEQ, dtype=np.int64)
    ph = (np.outer(s, s) % SEQ).astype(np.float64) * (2.0 * np.pi / SEQ)
    def blk(m):
        m = m.astype(np.float32).reshape(TPS, 128, TPS, 128).transpose(2, 1, 0, 3)
        return np.ascontiguousarray(m.reshape(TPS, 128, TPS * 128)).astype(bf)
    dftc = blk(np.cos(ph))
    dfts = blk(np.sin(ph))
    c = np.arange(64, dtype=np.int64)
    ph64 = (np.outer(c, c) % 64).astype(np.float64) * (2.0 * np.pi / 64)
    nrm = 1.0 / np.sqrt(SEQ * 64.0)
    cc = np.cos(ph64) * nrm
    sc = np.sin(ph64) * nrm
    ccb = np.zeros((128, 128), np.float32)
    scb = np.zeros((128, 128), np.float32)
    for g in range(2):
        ccb[g * 64:(g + 1) * 64, g * 64:(g + 1) * 64] = cc
        scb[g * 64:(g + 1) * 64, g * 64:(g + 1) * 64] = sc
    inv_freq = np.power(np.float32(500000.0), -np.arange(0, 16, 2, dtype=np.float32) / np.float32(16))
    ang = np.arange(SEQ, dtype=np.float32)[:, None] * inv_freq[None, :]
    a = np.arange(128)
    consts = {
        "dftc": dftc, "dfts": dfts, "ccb": ccb, "scb": scb,
        "rope_c": np.cos(ang).astype(np.float32), "rope_s": np.sin(ang).astype(np.float32),
        "k_ident": np.eye(128, dtype=np.float32),
        "k_lstrict": (a[:, None] < a[None, :]).astype(np.float32),
        "k_maska": (a[None, :] <= a[:, None]).astype(np.float32),
        "k_maskb": (a[:, None] <= a[None, :]).astype(np.float32),
    }
    return consts


_NC_CACHE = {}


def run(x_all, c_all, weights, NL, NSEQ, C, n_cores, stage="full"):
    key = (NL, NSEQ, C, stage)
    if key not in _NC_CACHE:
        _NC_CACHE[key] = build(NL, NSEQ, C, stage)
    nc = _NC_CACHE[key]
    consts = make_consts()
    consts["k_ebase"] = np.tile((np.arange(NE, dtype=np.float32) * C)[None, :], (128, 1)).astype(np.float32)
    consts["k_trash"] = (NE * C + 1 + np.arange(128, dtype=np.float32)).reshape(128, 1)
    shared = dict(consts)
    for k, v in weights.items():
        v = np.ascontiguousarray(np.asarray(v, dtype=np.float32))
        if k == "final_g":
            v = v.reshape(1, D)
        else:
            v = v[:NL]
        shared[k] = v
    in_maps = []
    for ci in range(n_cores):
        m = dict(shared)
        xs = x_all[ci * NSEQ:(ci + 1) * NSEQ]
        m["x_in"] = np.ascontiguousarray(xs.reshape(NSEQ * SEQ, D))
        m["cT"] = np.ascontiguousarray(c_all[ci * NSEQ:(ci + 1) * NSEQ].T)
        in_maps.append(m)
    res = run_bass_kernel_spmd(nc, in_maps, core_ids=list(range(n_cores)))
    outs = [np.asarray(r["y"]).reshape(NSEQ, SEQ, D) for r in res.results]
    if stage == "full":
        try:
            cnts = np.stack([np.asarray(r["dbg"])[0].reshape(NL, NE) for r in res.results])
            print("expert-count max per layer (capacity %d):" % C, cnts.max(axis=(0, 2)).tolist(),
                  "overflow assignments per layer:", np.maximum(cnts - C, 0).sum(axis=(0, 2)).tolist(), flush=True)
        except Exception as ex:
            print("dbg unavailable", ex)
    return np.concatenate(outs, axis=0)


CAPACITY = 3072


def kernel(x_prompt, x_sample, c_prompt, c_sample, norm1_g, ada_w, ada_b, w_in, b_in, w_fmix, sinks, w_o, b_o,
           norm2_g, w_router, b_router, w_gu, b_gu, w_down, b_down, final_g):
    x_prompt = np.asarray(x_prompt, dtype=np.float32)
    x_sample = np.asarray(x_sample, dtype=np.float32)
    c_prompt = np.asarray(c_prompt, dtype=np.float32)
    c_sample = np.asarray(c_sample, dtype=np.float32)
    n_cores = 8
    pp = x_prompt.shape[0] // n_cores
    sp = x_sample.shape[0] // n_cores
    xs, cs = [], []
    for ci in range(n_cores):
        xs.append(x_prompt[ci * pp:(ci + 1) * pp])
        xs.append(x_sample[ci * sp:(ci + 1) * sp])
        cs.append(c_prompt[ci * pp:(ci + 1) * pp])
        cs.append(c_sample[ci * sp:(ci + 1) * sp])
    x_all = np.concatenate(xs, axis=0)
    c_all = np.concatenate(cs, axis=0)
    weights = dict(norm1_g=norm1_g, ada_w=ada_w, ada_b=ada_b, w_in=w_in, b_in=b_in, w_fmix=w_fmix, sinks=sinks,
                   w_o=w_o, b_o=b_o, norm2_g=norm2_g, w_router=w_router, b_router=b_router, w_gu=w_gu, b_gu=b_gu,
                   w_down=w_down, b_down=b_down, final_g=final_g)
    out = run(x_all, c_all, weights, 4, pp + sp, CAPACITY, n_cores)
    nseq = pp + sp
    yp = np.concatenate([out[ci * nseq:ci * nseq + pp] for ci in range(n_cores)], axis=0)
    ys = np.concatenate([out[ci * nseq + pp:(ci + 1) * nseq] for ci in range(n_cores)], axis=0)
    return (np.ascontiguousarray(yp, dtype=np.float32), np.ascontiguousarray(ys, dtype=np.float32))
```

```python
import contextlib
import numpy as np
import ml_dtypes
import concourse.bass as bass
import concourse.mybir as mybir
from concourse.bass_utils import run_bass_kernel_spmd

F32 = mybir.dt.float32
BF16 = mybir.dt.bfloat16
I32 = mybir.dt.int32
AF = mybir.ActivationFunctionType
ALU = mybir.AluOpType

SAME_ENGINE_SYNC = True

D = 1024
SEQ = 2048
TPS = 16
NE = 32
DFF = 512
INW = 1536
EPS = 1e-5


class _Stop(Exception):
    pass


class Res:
    __slots__ = ("name", "w", "r", "excl")

    def __init__(self, name="", excl=False):
        self.name = name
        self.w = None
        self.r = []
        self.excl = excl


class Sched:
    ENGS = ("pe", "act", "dve", "pool", "sp")

    def __init__(self, nc, stack, n_dma_sems=48):
        self.nc = nc
        self.streams = {e: [] for e in self.ENGS}
        self.count = {e: 0 for e in self.ENGS}
        self.pending = {e: False for e in self.ENGS}
        self.waited = {e: {} for e in self.ENGS}
        self.sems = {}
        for e in self.ENGS:
            self.sems["E_" + e] = stack.enter_context(nc.semaphore("sem_" + e))
        self.dma_sems = []
        for i in range(n_dma_sems):
            k = "D_%d" % i
            self.sems[k] = stack.enter_context(nc.semaphore("semd_%d" % i))
            self.dma_sems.append(k)
        self.dma_val = {k: 0 for k in self.dma_sems}
        self.dma_rr = 0
        self.dma_rr_sw = 0
        self.n_inst = 0

    def _need(self, eng, tok):
        if tok is None:
            return
        key, val, teng = tok
        if teng == eng:
            if eng == "pe" or not SAME_ENGINE_SYNC or val > self.count[eng]:
                return
        if self.waited[eng].get(key, 0) >= val:
            return
        self.waited[eng][key] = val
        self.streams[eng].append(("wait", key, val))

    def _deps(self, eng, reads, writes):
        for r in reads:
            self._need(eng, r.w)
        for w in writes:
            self._need(eng, w.w)
            for t in w.r:
                self._need(eng, t)

    def _commit(self, tok, reads, writes):
        for r in reads:
            r.r.append(tok)
            if len(r.r) > 48:
                best = {}
                for t in r.r:
                    if t[0] not in best or best[t[0]][1] < t[1]:
                        best[t[0]] = t
                r.r = list(best.values())
        for w in writes:
            w.w = tok
            w.r = []

    def op(self, eng, fn, reads=(), writes=(), signal=True):
        if any(r.excl for r in reads):
            writes = list(writes) + [r for r in reads if r.excl]
            reads = [r for r in reads if not r.excl]
        self._deps(eng, reads, writes)
        if signal:
            self.count[eng] += 1
            val = self.count[eng]
            self.streams[eng].append(("inst", fn, "E_" + eng))
            self.pending[eng] = False
        else:
            val = self.count[eng] + 1
            self.streams[eng].append(("inst", fn, None))
            self.pending[eng] = True
        tok = ("E_" + eng, val, eng)
        self._commit(tok, reads, writes)
        self.n_inst += 1
        return tok

    def dma(self, eng, fn, reads=(), writes=()):
        self._deps(eng, reads, writes)
        half = len(self.dma_sems) // 2
        if eng == "pool":
            key = self.dma_sems[self.dma_rr_sw % half]
            self.dma_rr_sw += 1
        else:
            key = self.dma_sems[half + self.dma_rr % half]
            self.dma_rr += 1
        prev = self.dma_val[key]
        if prev > 0:
            self._need(eng, (key, prev, None))
        self.dma_val[key] = prev + 16
        self.streams[eng].append(("dma", fn, key))
        tok = (key, prev + 16, None)
        self._commit(tok, reads, writes)
        self.n_inst += 1
        return tok

    def barrier(self):
        toks = []
        for e in self.ENGS:
            if self.pending[e]:
                raise RuntimeError("pending unsignalled instrs on " + e)
            if self.count[e] > 0:
                toks.append(("E_" + e, self.count[e], e))
        for k in self.dma_sems:
            if self.dma_val[k] > 0:
                toks.append((k, self.dma_val[k], None))
        for e in self.ENGS:
            for t in toks:
                if t[2] == e:
                    continue
                self._need(e, t)

    def final_wait(self, eng="sp"):
        for k in self.dma_sems:
            if self.dma_val[k] > 0:
                self._need(eng, (k, self.dma_val[k], None))
        for e in self.ENGS:
            if e != eng and self.count[e] > 0:
                self._need(eng, ("E_" + e, self.count[e], e))

    def emit(self):
        nc = self.nc
        sems = self.sems
        streams = self.streams

        def run(engobj, items):
            for it in items:
                if it[0] == "wait":
                    engobj.wait_ge(sems[it[1]], it[2])
                elif it[0] == "inst":
                    ins = it[1](engobj)
                    if it[2] is not None:
                        ins.then_inc(sems[it[2]], 1)
                else:
                    ins = it[1](engobj)
                    ins.then_inc(sems[it[2]], 16)

        with nc.Block() as block:
            @block.tensor
            def _(e):
                run(e, streams["pe"])

            @block.scalar
            def _(e):
                run(e, streams["act"])

            @block.vector
            def _(e):
                run(e, streams["dve"])

            @block.gpsimd
            def _(e):
                run(e, streams["pool"])

            @block.sync
            def _(e):
                run(e, streams["sp"])


class Arena:
    def __init__(self, ap_f32, nwords):
        self.ap = ap_f32
        self.n = nwords
        self.top = 0

    def mark(self):
        return self.top

    def release(self, m):
        self.top = m

    def alloc(self, free_shape, dtype=F32, parts=128):
        nel = 1
        for s in free_shape:
            nel *= s
        esz = 4 if dtype in (F32, I32) else 2
        nw = (nel * esz + 3) // 4
        nw = (nw + 7) // 8 * 8
        if self.top + nw > self.n:
            raise RuntimeError("SBUF arena overflow: need %d words, top %d, cap %d" % (nw, self.top, self.n))
        a = self.ap[0:parts, self.top:self.top + nw]
        self.top += nw
        if dtype != F32:
            a = a.bitcast(dtype)
        a = a[:, 0:nel]
        if len(free_shape) == 2:
            a = a.rearrange("p (a b) -> p a b", a=free_shape[0])
        elif len(free_shape) == 3:
            a = a.rearrange("p (a b c) -> p a b c", a=free_shape[0], b=free_shape[1])
        return a


Q_PAIRS = [(0, 3), (1, 4), (2, 5), (6, 9), (7, 10), (8, 11)]


def build(NL, NSEQ, C, stage="full"):
    T = NSEQ * SEQ
    NT = T // 128
    NG = C // 512
    nc = bass.Bass("TRN2", target_bir_lowering=False)

    def din(name, shape, dt=F32):
        return nc.dram_tensor(name, list(shape), dt, kind="ExternalInput").ap()

    x_in = din("x_in", [T, D])
    cT = din("cT", [D, NSEQ])
    norm1_g = din("norm1_g", [NL, D])
    ada_w = din("ada_w", [NL, D, 6 * D])
    ada_b = din("ada_b", [NL, 6 * D])
    w_in = din("w_in", [NL, D, INW])
    b_in = din("b_in", [NL, INW])
    w_fmix = din("w_fmix", [NL, 4, 64, 64])
    sinks = din("sinks", [NL, 12])
    w_o = din("w_o", [NL, D, D])
    b_o = din("b_o", [NL, D])
    norm2_g = din("norm2_g", [NL, D])
    w_router = din("w_router", [NL, D, NE])
    b_router = din("b_router", [NL, NE])
    w_gu = din("w_gu", [NL, NE, D, 2 * DFF])
    b_gu = din("b_gu", [NL, NE, 2 * DFF])
    w_down = din("w_down", [NL, NE, DFF, D])
    b_down = din("b_down", [NL, NE, D])
    final_g = din("final_g", [1, D])
    dftc = din("dftc", [TPS, 128, TPS * 128], BF16)
    dfts = din("dfts", [TPS, 128, TPS * 128], BF16)
    ccb = din("ccb", [128, 128])
    scb = din("scb", [128, 128])
    rope_c = din("rope_c", [SEQ, 8])
    rope_s = din("rope_s", [SEQ, 8])
    k_ident = din("k_ident", [128, 128])
    k_lstrict = din("k_lstrict", [128, 128])
    k_maska = din("k_maska", [128, 128])
    k_maskb = din("k_maskb", [128, 128])
    k_ebase = din("k_ebase", [128, NE])
    k_trash = din("k_trash", [128, 4])

    y = nc.dram_tensor("y", [T, D], F32, kind="ExternalOutput").ap()
    dbg = nc.dram_tensor("dbg", [128, NL * NE], F32, kind="ExternalOutput").ap()
    xres = nc.dram_tensor("xres", [T, D], F32, kind="Internal").ap()
    Xs = nc.dram_tensor("Xs", [NE * C + 4096, D], BF16, kind="Internal").ap()
    Yd = nc.dram_tensor("Yd", [NE * C, D], BF16, kind="Internal").ap()
    modscr = nc.dram_tensor("modscr", [NSEQ, 7, D], F32, kind="Internal").ap()

    with contextlib.ExitStack() as st:
        S = Sched(nc, st)
        ARENA_WORDS = 53120 - 2 * NT * 4 - 16
        arena_t = st.enter_context(nc.sbuf_tensor("arena", [128, ARENA_WORDS], F32))
        AR = Arena(arena_t, ARENA_WORDS)
        ps = [st.enter_context(nc.psum_tensor("psb%d" % i, [128, 512], F32)) for i in range(8)]
        psr = [Res("ps%d" % i, excl=True) for i in range(8)]

        def ps_bf(i):
            return ps[i][:].bitcast(BF16)

        ident_f = AR.alloc([128])
        ident_b = AR.alloc([128], BF16)
        lstrict_b = AR.alloc([128], BF16)
        ones_b = AR.alloc([128], BF16)
        maska_b = AR.alloc([128], BF16)
        maskb_b = AR.alloc([128], BF16)
        ebase = AR.alloc([NE])
        trashc = AR.alloc([4])
        run_cnt = AR.alloc([NE])
        dbg_sb = AR.alloc([NL * NE])
        r_dbg = Res()
        ropec = AR.alloc([TPS, 8])
        ropes = AR.alloc([TPS, 8])
        tmpc = AR.alloc([128])
        slot_sc_t = st.enter_context(nc.sbuf_tensor("slot_sc", [128, NT * 4], I32))
        slot_g_t = st.enter_context(nc.sbuf_tensor("slot_g", [128, NT * 4], I32))
        slot_sc_all = slot_sc_t[:, :].rearrange("p (t k) -> p t k", k=4)
        slot_g_all = slot_g_t[:, :].rearrange("p (t k) -> p t k", k=4)
        gate_all = AR.alloc([NT, 4])
        siluT = AR.alloc([8, NSEQ])
        siluT_r = Res()
        r_const = Res("const")
        r_run = Res("run")
        r_slots = Res("slots")
        r_slot_t = [Res() for _ in range(NT)]

        def ld(dst, src, res, eng="sp"):
            S.dma(eng, lambda e: e.dma_start(out=dst, in_=src), writes=[res])

        ld(ident_f, k_ident, r_const)
        S.op("dve", lambda e: e.tensor_copy(out=ident_b, in_=ident_f), reads=[r_const], writes=[r_const])
        rr = Res()
        for (dst, src) in ((lstrict_b, k_lstrict), (maska_b, k_maska), (maskb_b, k_maskb)):
            ld(tmpc, src, rr)
            S.op("dve", lambda e, dst=dst: e.tensor_copy(out=dst, in_=tmpc), reads=[rr], writes=[r_const, rr])
        S.op("pool", lambda e: e.memset(ones_b, 1.0), writes=[r_const])
        ld(ebase, k_ebase, r_const)
        ld(trashc, k_trash, r_const)
        ld(ropec, rope_c.rearrange("(t p) i -> p t i", p=128), r_const)
        ld(ropes, rope_s.rearrange("(t p) i -> p t i", p=128), r_const)
        S.dma("sp", lambda e: e.dma_start(out=siluT, in_=cT.rearrange("(c p) s -> p c s", p=128),
                                          allow_slow_non_contiguous=True), writes=[siluT_r])
        S.op("act", lambda e: e.activation(out=siluT, in_=siluT, func=AF.Silu), reads=[siluT_r], writes=[siluT_r])

        w_in_sb = AR.alloc([8, INW], BF16)
        w_o_sb = AR.alloc([8, D], BF16)
        wr_sb = AR.alloc([8, NE], BF16)
        brow = AR.alloc([INW + NE], BF16, parts=1)
        ablk = AR.alloc([2, 128], BF16)
        nbblk = AR.alloc([2, 128], BF16)
        bguT = AR.alloc([8, NE])
        expsink = AR.alloc([12])
        r_win, r_wo, r_wr, r_brow, r_ablk, r_bgu, r_sink = (Res() for _ in range(7))

        phase_mark = AR.mark()
        if stage not in ("A", "A0", "A1", "A3"):
            zt = AR.alloc([4, D], BF16)
            r_zt = Res()
            S.op("pool", lambda e: e.memset(zt, 0.0), writes=[r_zt])
            for r0 in range(0, NE * C, 512):
                S.dma("sp", lambda e, r0=r0: e.dma_start(out=Xs[r0:r0 + 512, :].rearrange("(t p) d -> p t d", p=128),
                                                        in_=zt), reads=[r_zt])
            for r0 in range(NE * C, NE * C + 4096, 512):
                S.dma("sp", lambda e, r0=r0: e.dma_start(out=Xs[r0:r0 + 512, :].rearrange("(t p) d -> p t d", p=128),
                                                        in_=zt), reads=[r_zt])
            S.barrier()
            AR.release(phase_mark)

        def xsrc(l):
            return x_in if l == 0 else y

        for l in range(NL):
          try:
              last_layer = (l == NL - 1)
              S.dma("pool", lambda e, l=l: e.dma_start(out=w_in_sb, in_=w_in[l].rearrange("(c p) f -> p c f", p=128)),
                    writes=[r_win])
              S.dma("pool", lambda e, l=l: e.dma_start(out=w_o_sb, in_=w_o[l].rearrange("(c p) f -> p c f", p=128)),
                    writes=[r_wo])
              S.dma("pool", lambda e, l=l: e.dma_start(out=wr_sb, in_=w_router[l].rearrange("(c p) f -> p c f", p=128)),
                    writes=[r_wr])
              S.dma("pool", lambda e, l=l: e.dma_start(out=brow[:, 0:INW], in_=b_in[l:l + 1, :]), writes=[r_brow])
              S.dma("pool", lambda e, l=l: e.dma_start(out=brow[:, INW:INW + NE], in_=b_router[l:l + 1, :]),
                    writes=[r_brow])

              m0 = AR.mark()
              adaw = [AR.alloc([8, 512]) for _ in range(2)]
              r_adaw = [Res(), Res()]
              modblk = [AR.alloc([512], parts=NSEQ) for _ in range(2)]
              r_modblk = [Res(), Res()]
              gainb = AR.alloc([3, D], parts=NSEQ)
              adab = AR.alloc([6 * D], parts=NSEQ)
              r_gain = Res()
              g1keep = AR.alloc([D], parts=NSEQ)
              r_g1keep = Res()
              for i, src in enumerate((norm1_g, norm2_g, b_o)):
                  S.dma("sp", lambda e, i=i, src=src, l=l: e.dma_start(
                      out=gainb[:, i, :], in_=src[l:l + 1, :].partition_broadcast(NSEQ)), writes=[r_gain])
              S.dma("sp", lambda e, l=l: e.dma_start(out=adab, in_=ada_b[l:l + 1, :].partition_broadcast(NSEQ)),
                    writes=[r_gain])
              for cb in range(12):
                  b = cb % 2
                  m = cb // 2
                  hcol = (cb % 2) * 512
                  S.dma("sp", lambda e, l=l, cb=cb, b=b: e.dma_start(
                      out=adaw[b], in_=ada_w[l, :, cb * 512:(cb + 1) * 512].rearrange("(c p) f -> p c f", p=128)),
                      writes=[r_adaw[b]])
                  for kc in range(8):
                      S.op("pe", lambda e, kc=kc, b=b: e.matmul(ps[0][0:NSEQ, :], lhsT=siluT[:, kc, :], rhs=adaw[b][:, kc, :],
                                                               start=(kc == 0), stop=(kc == 7)),
                           reads=[siluT_r, r_adaw[b]], writes=[psr[0]], signal=(kc == 7))
                  mb = modblk[b]
                  S.op("dve", lambda e, mb=mb, cb=cb: e.tensor_tensor(out=mb, in0=ps[0][0:NSEQ, :],
                                                                       in1=adab[:, cb * 512:(cb + 1) * 512], op=ALU.add),
                       reads=[psr[0], r_gain], writes=[r_modblk[b]])
                  if m in (1, 4):
                      gi = 0 if m == 1 else 1
                      S.op("dve", lambda e, mb=mb, gi=gi, hcol=hcol: e.scalar_tensor_tensor(
                          out=mb, in0=mb, scalar=1.0, in1=gainb[:, gi, hcol:hcol + 512], op0=ALU.add, op1=ALU.mult),
                          reads=[r_gain], writes=[r_modblk[b]])
                  dslot = {0: 1, 1: 0, 2: 2, 3: 5, 4: 4, 5: 6}[m]
                  S.dma("sp", lambda e, mb=mb, dslot=dslot, hcol=hcol: e.dma_start(
                      out=modscr[:, dslot, hcol:hcol + 512], in_=mb), reads=[r_modblk[b]])
                  if m == 2:
                      S.op("dve", lambda e, mb=mb, hcol=hcol: e.tensor_tensor(
                          out=g1keep[:, hcol:hcol + 512], in0=mb, in1=gainb[:, 2, hcol:hcol + 512], op=ALU.mult),
                          reads=[r_modblk[b], r_gain], writes=[r_g1keep])
              S.dma("sp", lambda e: e.dma_start(out=modscr[:, 3, :], in_=g1keep), reads=[r_g1keep])

              ccs = AR.alloc([2, 128])
              wblk = AR.alloc([2, 128])
              r_ccs, r_wblk = Res(), Res()
              ld(ccs[:, 0, :], ccb, r_ccs)
              ld(ccs[:, 1, :], scb, r_ccs)
              S.op("pool", lambda e: e.memset(wblk, 0.0), writes=[r_wblk])
              for cc in range(2):
                  for gg in range(2):
                      S.dma("sp", lambda e, l=l, cc=cc, gg=gg: e.dma_start(
                          out=wblk[gg * 64:(gg + 1) * 64, cc, gg * 64:(gg + 1) * 64], in_=w_fmix[l, 2 * cc + gg]),
                          writes=[r_wblk])
              for cc in range(2):
                  for t in range(2):
                      S.op("pe", lambda e, cc=cc, t=t: e.matmul(ps[1][:, t * 256 + cc * 128:t * 256 + (cc + 1) * 128],
                                                               lhsT=ccs[:, t, :], rhs=wblk[:, cc, :], start=True, stop=True),
                           reads=[r_ccs, r_wblk], writes=[psr[1]])
              S.op("act", lambda e: e.activation(out=ablk, in_=ps[1][:, 0:256].rearrange("p (a b) -> p a b", a=2),
                                                 func=AF.Identity), reads=[psr[1]], writes=[r_ablk])
              S.op("act", lambda e: e.activation(out=nbblk, in_=ps[1][:, 256:512].rearrange("p (a b) -> p a b", a=2),
                                                 func=AF.Identity, scale=-1.0), reads=[psr[1]], writes=[r_ablk])
              bgr = AR.alloc([2 * DFF], parts=NE)
              r_bgr = Res()
              ld(bgr, b_gu[l], r_bgr)
              for c8 in range(8):
                  S.op("pe", lambda e, c8=c8: e.transpose(out=ps[2][:, c8 * NE:(c8 + 1) * NE],
                                                          in_=bgr[:, c8 * 128:(c8 + 1) * 128], identity=ident_f[0:NE, 0:NE]),
                       reads=[r_bgr, r_const], writes=[psr[2]], signal=(c8 == 7))
              S.op("act", lambda e: e.activation(out=bguT, in_=ps[2][:, 0:8 * NE].rearrange("p (a b) -> p a b", a=8),
                                                 func=AF.Identity), reads=[psr[2]], writes=[r_bgu])
              S.dma("sp", lambda e, l=l: e.dma_start(out=expsink, in_=sinks[l:l + 1, :].partition_broadcast(128)),
                    writes=[r_sink])
              S.op("act", lambda e: e.activation(out=expsink, in_=expsink, func=AF.Exp), reads=[r_sink], writes=[r_sink])
              S.op("pool", lambda e: e.memset(run_cnt, 0.0), writes=[r_run])
              S.barrier()
              AR.release(m0)
              if stage == 'A0':
                  raise _Stop()

              mA = AR.mark()
              bcf = AR.alloc([6, D])
              r_bc = Res()
              xt = [AR.alloc([D]) for _ in range(2)]
              r_xt = [Res(), Res()]
              tmpf = AR.alloc([D])
              r_tmpf = Res()
              sq = tmpf
              r_sq = r_tmpf
              hb = [AR.alloc([D], BF16) for _ in range(2)]
              r_hb = [Res(), Res()]
              hT = [AR.alloc([8, 128], BF16) for _ in range(2)]
              r_hT = [Res(), Res()]
              stat = AR.alloc([16])
              r_stat = Res()
              f_tm = AR.alloc([TPS, 256], BF16)
              r_f = Res()
              v_aug = AR.alloc([TPS, 4, 66], BF16)
              r_v = Res()
              qk_tm = [AR.alloc([1024], BF16) for _ in range(2)]
              r_qk = [Res(), Res()]
              ropet = AR.alloc([4, 16, 8])
              r_ropet = Res()
              QT = AR.alloc([6, SEQ], BF16)
              KT = AR.alloc([2, SEQ], BF16)
              r_QT, r_KT = Res(), Res()
              dftb = [[AR.alloc([TPS, 128], BF16) for _ in range(2)] for _ in range(2)]
              r_dft = [Res(), Res()]
              ysb = [AR.alloc([2, 2, 128], BF16) for _ in range(2)]
              r_ysb = [Res(), Res()]
              foT = AR.alloc([2, SEQ], BF16)
              r_foT = Res()
              PT = [AR.alloc([3, 128], BF16) for _ in range(6)]
              r_PT = [Res() for _ in range(6)]
              den = AR.alloc([12])
              r_den = Res()
              attn_tm = [AR.alloc([12, 64], BF16) for _ in range(2)]
              r_attn = [Res(), Res()]
              attnT = [AR.alloc([6, 128], BF16) for _ in range(2)]
              r_attnT = [Res(), Res()]
              xb = AR.alloc([D])
              r_xb = Res()
              xmid_ = AR.alloc([D])
              xmid = [xmid_, xmid_]
              r_xmid_ = Res()
              r_xmid = [r_xmid_, r_xmid_]
              h2 = [AR.alloc([D], BF16) for _ in range(2)]
              r_h2 = [Res(), Res()]
              h2T = AR.alloc([8, 128], BF16)
              r_h2T = Res()
              rt_lg = AR.alloc([NE])
              rt_t8 = AR.alloc([8])
              rt_mask = AR.alloc([NE])
              rt_maskb = AR.alloc([NE], BF16)
              rt_e = AR.alloc([NE])
              rt_G = AR.alloc([NE])
              rt_cnt = AR.alloc([NE])
              rt_ms = AR.alloc([NE])
              rt_s8 = AR.alloc([8])
              rt_small = AR.alloc([16])
              rt_junk = AR.alloc([NE])
              r_rt = Res()
              S.op("pool", lambda e: e.memset(v_aug, 1.0), writes=[r_v])

              def rms_mod(xtile, r_x, a_idx, s_idx, out_b, r_out):
                  S.op("act", lambda e: e.activation(out=sq, in_=xtile, func=AF.Square, accum_out=stat[:, 0:1]),
                       reads=[r_x], writes=[r_sq, r_stat])
                  S.op("act", lambda e: e.activation(out=stat[:, 1:2], in_=stat[:, 0:1], func=AF.Sqrt,
                                                     scale=1.0 / D, bias=EPS), reads=[r_stat], writes=[r_stat])
                  S.op("dve", lambda e: e.reciprocal(out=stat[:, 2:3], in_=stat[:, 1:2]), reads=[r_stat], writes=[r_stat])
                  S.op("dve", lambda e: e.scalar_tensor_tensor(out=tmpf, in0=xtile, scalar=stat[:, 2:3],
                                                               in1=bcf[:, a_idx, :], op0=ALU.mult, op1=ALU.mult),
                       reads=[r_x, r_stat, r_bc], writes=[r_tmpf])
                  S.op("dve", lambda e: e.tensor_tensor(out=out_b, in0=tmpf, in1=bcf[:, s_idx, :], op=ALU.add),
                       reads=[r_tmpf, r_bc], writes=[r_out])

              def transpose8(src_b, r_src, bank, dst, r_dst, evac_eng):
                  pv = ps_bf(bank).rearrange("p (c t) -> p c t", c=8)
                  for c in range(8):
                      S.op("pe", lambda e, c=c: e.transpose(out=pv[:, c, :], in_=src_b[:, c * 128:(c + 1) * 128],
                                                            identity=ident_b), reads=[r_src, r_const],
                           writes=[psr[bank]], signal=(c == 7))
                  if evac_eng == "act":
                      S.op("act", lambda e: e.activation(out=dst, in_=pv, func=AF.Identity),
                           reads=[psr[bank]], writes=[r_dst])
                  else:
                      S.op("dve", lambda e: e.tensor_copy(out=dst, in_=pv), reads=[psr[bank]], writes=[r_dst])

              for s in range(NSEQ):
                  S.dma("sp", lambda e, s=s: e.dma_start(
                      out=bcf, in_=modscr[s:s + 1, 0:6, :].rearrange("o a d -> o (a d)").partition_broadcast(128)),
                      writes=[r_bc])
                  def stA(tt, s=s, l=l):
                      gt = s * TPS + tt
                      b = tt % 2
                      S.dma("sp", lambda e, gt=gt, b=b, l=l: e.dma_start(out=xt[b], in_=xsrc(l)[gt * 128:(gt + 1) * 128, :]),
                            writes=[r_xt[b]])
                      rms_mod(xt[b], r_xt[b], 0, 1, hb[b], r_hb[b])
                      transpose8(hb[b], r_hb[b], 7, hT[b], r_hT[b], "act")

                  def stB(tt, s=s, l=l):
                      gt = s * TPS + tt
                      b = tt % 2
                      for cb in range(3):
                          for kc in range(8):
                              S.op("pe", lambda e, cb=cb, kc=kc, b=b: e.matmul(
                                  ps[cb][:], lhsT=hT[b][:, kc, :], rhs=w_in_sb[:, kc, cb * 512:(cb + 1) * 512],
                                  start=(kc == 0), stop=False), reads=[r_hT[b], r_win], writes=[psr[cb]], signal=False)
                          S.op("pe", lambda e, cb=cb: e.matmul(
                              ps[cb][:], lhsT=ones_b[0:1, :], rhs=brow[0:1, cb * 512:(cb + 1) * 512],
                              start=False, stop=True), reads=[r_const, r_brow], writes=[psr[cb]])
                      S.op("act", lambda e, tt=tt: e.activation(out=f_tm[:, tt, :], in_=ps[0][:, 0:256], func=AF.Identity),
                           reads=[psr[0]], writes=[r_f])
                      qb = qk_tm[b]
                      zst = xt[b]
                      rz = r_xt[b]
                      S.op("act", lambda e, zst=zst: e.activation(out=zst[:, 0:256], in_=ps[0][:, 256:512], func=AF.Identity),
                           reads=[psr[0]], writes=[rz])
                      S.op("act", lambda e, zst=zst: e.activation(out=zst[:, 256:768], in_=ps[1][:], func=AF.Identity),
                           reads=[psr[1]], writes=[rz])
                      S.op("act", lambda e, zst=zst: e.activation(out=zst[:, 768:1024], in_=ps[2][:, 0:256], func=AF.Identity),
                           reads=[psr[2]], writes=[rz])
                      S.op("act", lambda e, tt=tt: e.activation(
                          out=v_aug[:, tt, :, 0:64], in_=ps[2][:, 256:512].rearrange("p (h d) -> p h d", h=4),
                          func=AF.Identity), reads=[psr[2]], writes=[r_v])
                      cosb = ropec[:, tt, :].unsqueeze(1).to_broadcast([128, 16, 8])
                      sinb = ropes[:, tt, :].unsqueeze(1).to_broadcast([128, 16, 8])
                      zv = zst.rearrange("p (h d) -> p h d", h=16)
                      x1 = zv[:, :, 0:8]
                      x2 = zv[:, :, 8:16]
                      t0, t1, t2, t3 = (ropet[:, i, :, :] for i in range(4))
                      S.op("dve", lambda e, t0=t0, x1=x1, cosb=cosb: e.tensor_tensor(out=t0, in0=x1, in1=cosb, op=ALU.mult),
                           reads=[rz, r_const], writes=[r_ropet])
                      S.op("dve", lambda e, t1=t1, x2=x2, sinb=sinb: e.tensor_tensor(out=t1, in0=x2, in1=sinb, op=ALU.mult),
                           reads=[rz, r_const], writes=[r_ropet])
                      S.op("dve", lambda e, t2=t2, x2=x2, cosb=cosb: e.tensor_tensor(out=t2, in0=x2, in1=cosb, op=ALU.mult),
                           reads=[rz, r_const], writes=[r_ropet])
                      S.op("dve", lambda e, t3=t3, x1=x1, sinb=sinb: e.tensor_tensor(out=t3, in0=x1, in1=sinb, op=ALU.mult),
                           reads=[rz, r_const], writes=[r_ropet])
                      S.op("dve", lambda e, x1=x1, t0=t0, t1=t1: e.tensor_tensor(out=x1, in0=t0, in1=t1, op=ALU.subtract),
                           reads=[r_ropet], writes=[rz])
                      S.op("dve", lambda e, x2=x2, t2=t2, t3=t3: e.tensor_tensor(out=x2, in0=t2, in1=t3, op=ALU.add),
                           reads=[r_ropet], writes=[rz])
                      qdst = qb[:, 0:768].rearrange("p (h d) -> p h d", h=12)
                      for half6 in range(2):
                          src = zv[:, half6 * 6:(half6 + 1) * 6, :].rearrange("p (a b) d -> p a b d", a=2)
                          dst = qdst[:, half6 * 6:(half6 + 1) * 6, :].rearrange("p (b a) d -> p a b d", a=2)
                          S.op("pool", lambda e, src=src, dst=dst: e.tensor_copy(out=dst, in_=src), reads=[rz],
                               writes=[r_qk[b]])
                      S.op("act", lambda e, zst=zst, qb=qb: e.activation(out=qb[:, 768:1024], in_=zst[:, 768:1024],
                                                                         func=AF.Identity), reads=[rz], writes=[r_qk[b]])

                  def stC(tt, s=s, l=l):
                      gt = s * TPS + tt
                      b = tt % 2
                      qb = qk_tm[b]
                      pq = ps_bf(3).rearrange("p (c t) -> p c t", c=8)
                      for c in range(8):
                          S.op("pe", lambda e, c=c, qb=qb: e.transpose(out=pq[:, c, :], in_=qb[:, c * 128:(c + 1) * 128],
                                                                       identity=ident_b),
                               reads=[r_qk[b], r_const], writes=[psr[3]], signal=(c == 7))
                      S.op("act", lambda e, tt=tt: e.activation(out=QT[:, :, tt * 128:(tt + 1) * 128], in_=pq[:, 0:6, :],
                                                                func=AF.Identity), reads=[psr[3]], writes=[r_QT])
                      S.op("act", lambda e, tt=tt: e.activation(out=KT[:, :, tt * 128:(tt + 1) * 128], in_=pq[:, 6:8, :],
                                                                func=AF.Identity), reads=[psr[3]], writes=[r_KT])


                  for k_ in range(TPS + 2):
                      if k_ < TPS:
                          stA(k_)
                      if 0 <= k_ - 1 < TPS:
                          stB(k_ - 1)
                      if 0 <= k_ - 2 < TPS:
                          stC(k_ - 2)

                  if stage == 'A1':
                      raise _Stop()
                  for kb in range(SEQ // 128):
                      b = kb % 2
                      S.dma("sp", lambda e, kb=kb, b=b: e.dma_start(
                          out=dftb[b][0], in_=dftc[kb].rearrange("p (t k) -> p t k", t=TPS)),
                          writes=[r_dft[b]])
                      S.dma("sp", lambda e, kb=kb, b=b: e.dma_start(
                          out=dftb[b][1], in_=dfts[kb].rearrange("p (t k) -> p t k", t=TPS)),
                          writes=[r_dft[b]])
                      for t in range(2):
                          for cc in range(2):
                              for stile in range(TPS):
                                  S.op("pe", lambda e, t=t, cc=cc, stile=stile, b=b: e.matmul(
                                      ps[4][:, (t * 2 + cc) * 128:(t * 2 + cc + 1) * 128],
                                      lhsT=f_tm[:, stile, cc * 128:(cc + 1) * 128], rhs=dftb[b][t][:, stile, :],
                                      start=(stile == 0), stop=(stile == TPS - 1)),
                                      reads=[r_f, r_dft[b]], writes=[psr[4]], signal=(stile == TPS - 1))
                      S.op("act", lambda e, b=b: e.activation(
                          out=ysb[b], in_=ps[4][:].rearrange("p (a c k) -> p a c k", a=2, c=2), func=AF.Identity),
                          reads=[psr[4]], writes=[r_ysb[b]])
                      for cc in range(2):
                          S.op("pe", lambda e, cc=cc, b=b: e.matmul(ps[5][:, cc * 128:(cc + 1) * 128], lhsT=ablk[:, cc, :],
                                                                   rhs=ysb[b][:, 0, cc, :], start=True, stop=False),
                               reads=[r_ablk, r_ysb[b]], writes=[psr[5]], signal=False)
                          S.op("pe", lambda e, cc=cc, b=b: e.matmul(ps[5][:, cc * 128:(cc + 1) * 128], lhsT=nbblk[:, cc, :],
                                                                   rhs=ysb[b][:, 1, cc, :], start=False, stop=True),
                               reads=[r_ablk, r_ysb[b]], writes=[psr[5]])
                      S.op("dve", lambda e, kb=kb: e.tensor_copy(
                          out=foT[:, :, kb * 128:(kb + 1) * 128], in_=ps[5][:, 0:256].rearrange("p (c k) -> p c k", c=2)),
                          reads=[psr[5]], writes=[r_foT])

                  if stage == 'A3':
                      raise _Stop()
                  def stX(n, s=s, l=l):
                      ab = n % 2
                      js = [j for j in (n - 1, n, n + 1) if 0 <= j < TPS]
                      ptsg = {}

                      def qk_exp(g):
                          off = (g % 2) * 64
                          kc_ = g // 2
                          pts = []
                          for ji, j in enumerate(js):
                              sbank = 3 + (stX.cnt % 2)
                              stX.cnt += 1
                              pt_i = (g % 2) * 3 + ji
                              for i in range(3):
                                  S.op("pe", lambda e, i=i, j=j, off=off, kc_=kc_, sbank=sbank, n=n: e.matmul(
                                      ps[sbank][:, i * 128:(i + 1) * 128],
                                      lhsT=KT[off:off + 64, kc_, j * 128:(j + 1) * 128],
                                      rhs=QT[off:off + 64, kc_ * 3 + i, n * 128:(n + 1) * 128], start=True, stop=True),
                                      reads=[r_KT, r_QT], writes=[psr[sbank]], signal=(i == 2))
                              ptile = PT[pt_i]
                              S.op("act", lambda e, ptile=ptile, sbank=sbank: e.activation(
                                  out=ptile, in_=ps[sbank][:, 0:384].rearrange("p (i q) -> p i q", i=3), func=AF.Exp,
                                  scale=0.125), reads=[psr[sbank]], writes=[r_PT[pt_i]])
                              if j != n:
                                  mk = maska_b if j < n else maskb_b
                                  S.op("dve", lambda e, ptile=ptile, mk=mk: e.tensor_tensor(
                                      out=ptile, in0=ptile, in1=mk.unsqueeze(1).to_broadcast([128, 3, 128]), op=ALU.mult),
                                      reads=[r_const], writes=[r_PT[pt_i]])
                              pts.append((j, ptile, r_PT[pt_i]))
                          ptsg[g] = pts

                      def pv(g):
                          pts = ptsg[g]
                          for i in range(3):
                              h = 3 * g + i
                              obank = 5 + h // 6
                              ocol = (h % 6) * 65
                              for ji, (j, ptile, rpt) in enumerate(pts):
                                  S.op("pe", lambda e, i=i, j=j, g=g, ptile=ptile, obank=obank, ocol=ocol, ji=ji,
                                       nj=len(pts): e.matmul(
                                      ps[obank][:, ocol:ocol + 65], lhsT=ptile[:, i, :], rhs=v_aug[:, j, g, 0:65],
                                      start=(ji == 0), stop=(ji == nj - 1)),
                                      reads=[rpt, r_v], writes=[psr[obank]], signal=(ji == len(pts) - 1))

                      for g in range(5):
                          if g < 4:
                              qk_exp(g)
                          if g >= 1:
                              pv(g - 1)
                      for hb2 in range(2):
                          ov = ps[5 + hb2][:, 0:390].rearrange("p (h d) -> p h d", h=6)
                          S.op("dve", lambda e, ov=ov, hb2=hb2: e.tensor_tensor(
                              out=den[:, hb2 * 6:(hb2 + 1) * 6].unsqueeze(2), in0=ov[:, :, 64:65],
                              in1=expsink[:, hb2 * 6:(hb2 + 1) * 6].unsqueeze(2), op=ALU.add),
                              reads=[psr[5 + hb2], r_sink], writes=[r_den])
                          S.op("dve", lambda e, hb2=hb2: e.reciprocal(out=den[:, hb2 * 6:(hb2 + 1) * 6],
                                                                      in_=den[:, hb2 * 6:(hb2 + 1) * 6]),
                               reads=[r_den], writes=[r_den])
                          S.op("dve", lambda e, ov=ov, hb2=hb2, ab=ab: e.tensor_tensor(
                              out=attn_tm[ab][:, hb2 * 6:(hb2 + 1) * 6, :], in0=ov[:, :, 0:64],
                              in1=den[:, hb2 * 6:(hb2 + 1) * 6].unsqueeze(2).to_broadcast([128, 6, 64]), op=ALU.mult),
                              reads=[psr[5 + hb2], r_den], writes=[r_attn[ab]])
                      av = attn_tm[ab].rearrange("p h d -> p (h d)")
                      pa = ps_bf(7).rearrange("p (c t) -> p c t", c=8)
                      for c in range(6):
                          S.op("pe", lambda e, c=c, av=av: e.transpose(out=pa[:, c, :], in_=av[:, c * 128:(c + 1) * 128],
                                                                       identity=ident_b),
                               reads=[r_attn[ab], r_const], writes=[psr[7]], signal=(c == 5))
                      S.op("act", lambda e, ab=ab: e.activation(out=attnT[ab], in_=pa[:, 0:6, :], func=AF.Identity),
                           reads=[psr[7]], writes=[r_attnT[ab]])

                  stX.cnt = 0

                  def stY(n, s=s, l=l):
                      gt = s * TPS + n
                      ab = n % 2
                      rsl = r_slot_t[gt]
                      for half in range(2):
                          for kc in range(8):
                              if kc < 2:
                                  lhs = foT[:, kc, n * 128:(n + 1) * 128]
                                  rd = [r_foT, r_wo]
                              else:
                                  lhs = attnT[ab][:, kc - 2, :]
                                  rd = [r_attnT[ab], r_wo]
                              S.op("pe", lambda e, half=half, kc=kc, lhs=lhs: e.matmul(
                                  ps[half][:], lhsT=lhs, rhs=w_o_sb[:, kc, half * 512:(half + 1) * 512],
                                  start=(kc == 0), stop=(kc == 7)), reads=rd, writes=[psr[half]], signal=(kc == 7))
                      mb_ = n % 2
                      S.dma("sp", lambda e, gt=gt, l=l: e.dma_start(out=xb, in_=xsrc(l)[gt * 128:(gt + 1) * 128, :]),
                            writes=[r_xb])
                      S.op("pool", lambda e: e.tensor_tensor(out=xb, in0=xb, in1=bcf[:, 3, :], op=ALU.add),
                           reads=[r_bc], writes=[r_xb])
                      for half in range(2):
                          S.op("dve", lambda e, half=half: e.tensor_tensor(
                              out=tmpf[:, half * 512:(half + 1) * 512], in0=ps[half][:],
                              in1=bcf[:, 2, half * 512:(half + 1) * 512], op=ALU.mult),
                              reads=[psr[half], r_bc], writes=[r_tmpf])
                      S.op("dve", lambda e, mb_=mb_: e.tensor_tensor(out=xmid[mb_], in0=tmpf, in1=xb, op=ALU.add),
                           reads=[r_tmpf, r_xb], writes=[r_xmid[mb_]])
                      if stage == "A":
                          S.dma("sp", lambda e, gt=gt, mb_=mb_: e.dma_start(out=y[gt * 128:(gt + 1) * 128, :], in_=xmid[mb_]),
                                reads=[r_xmid[mb_]])
                          return
                      S.dma("sp", lambda e, gt=gt, mb_=mb_: e.dma_start(out=xres[gt * 128:(gt + 1) * 128, :], in_=xmid[mb_]),
                            reads=[r_xmid[mb_]])
                      rms_mod(xmid[mb_], r_xmid[mb_], 4, 5, h2[mb_], r_h2[mb_])

                  def stY2(n, s=s, l=l):
                      gt = s * TPS + n
                      mb_ = n % 2
                      rsl = r_slot_t[gt]
                      transpose8(h2[mb_], r_h2[mb_], 0, h2T, r_h2T, "act")
                      for kc in range(8):
                          S.op("pe", lambda e, kc=kc: e.matmul(ps[2][:, 0:NE], lhsT=h2T[:, kc, :], rhs=wr_sb[:, kc, :],
                                                               start=(kc == 0), stop=False),
                               reads=[r_h2T, r_wr], writes=[psr[2]], signal=False)
                      S.op("pe", lambda e: e.matmul(ps[2][:, 0:NE], lhsT=ones_b[0:1, :], rhs=brow[0:1, INW:INW + NE],
                                                    start=False, stop=True), reads=[r_const, r_brow], writes=[psr[2]])
                      R = [r_rt]
                      S.op("dve", lambda e: e.tensor_copy(out=rt_lg, in_=ps[2][:, 0:NE]), reads=[psr[2]], writes=R)
                      S.op("dve", lambda e: e.max(out=rt_t8, in_=rt_lg), writes=R)
                      S.op("dve", lambda e: e.tensor_scalar(out=rt_mask, in0=rt_lg, scalar1=rt_t8[:, 3:4], scalar2=None,
                                                            op0=ALU.is_ge), writes=R)
                      S.op("dve", lambda e: e.tensor_copy(out=rt_maskb, in_=rt_mask), writes=R)
                      S.op("dve", lambda e: e.tensor_scalar(out=rt_small[:, 0:1], in0=rt_t8[:, 0:1], scalar1=-1.0,
                                                            scalar2=None, op0=ALU.mult), writes=R)
                      S.op("act", lambda e: e.activation(out=rt_e, in_=rt_lg, func=AF.Exp, bias=rt_small[:, 0:1]),
                           reads=R, writes=R)
                      S.op("dve", lambda e: e.scalar_tensor_tensor(out=rt_G, in0=rt_e, scalar=1.0, in1=rt_mask,
                                                                   op0=ALU.mult, op1=ALU.mult,
                                                                   accum_out=rt_small[:, 1:2]), reads=R, writes=R)
                      S.op("dve", lambda e: e.reciprocal(out=rt_small[:, 2:3], in_=rt_small[:, 1:2]), writes=R)
                      S.op("dve", lambda e: e.tensor_scalar(out=rt_G, in0=rt_G, scalar1=rt_small[:, 2:3], scalar2=None,
                                                            op0=ALU.mult), writes=R)
                      S.op("pe", lambda e: e.matmul(ps[2][:, 64:64 + NE], lhsT=lstrict_b, rhs=rt_maskb, start=True, stop=True),
                           reads=[r_rt, r_const], writes=[psr[2]])
                      S.op("pe", lambda e: e.matmul(ps[2][:, 128:128 + NE], lhsT=ones_b, rhs=rt_maskb, start=True, stop=True),
                           reads=[r_rt, r_const], writes=[psr[2]])
                      S.op("dve", lambda e: e.tensor_tensor(out=rt_cnt, in0=ps[2][:, 64:64 + NE], in1=run_cnt, op=ALU.add),
                           reads=[psr[2], r_run], writes=R)
                      S.op("dve", lambda e: e.tensor_tensor(out=run_cnt, in0=ps[2][:, 128:128 + NE], in1=run_cnt, op=ALU.add),
                           reads=[psr[2]], writes=[r_run])
                      S.op("dve", lambda e: e.scalar_tensor_tensor(out=rt_ms, in0=rt_cnt, scalar=float(C), in1=rt_mask,
                                                                   op0=ALU.is_lt, op1=ALU.mult), writes=R)
                      S.op("dve", lambda e: e.tensor_tensor(out=rt_G, in0=rt_G, in1=rt_ms, op=ALU.mult), writes=R)
                      S.op("dve", lambda e: e.scalar_tensor_tensor(out=rt_cnt, in0=rt_cnt, scalar=1.0, in1=ebase,
                                                                   op0=ALU.add, op1=ALU.add), reads=[r_const], writes=R)
                      S.op("dve", lambda e: e.tensor_tensor(out=rt_ms, in0=rt_ms, in1=rt_cnt, op=ALU.mult), writes=R)
                      S.op("dve", lambda e: e.max(out=rt_s8, in_=rt_ms), writes=R)
                      for k in range(4):
                          S.op("dve", lambda e, k=k, gt=gt: e.scalar_tensor_tensor(
                              out=rt_junk, in0=rt_ms, scalar=rt_s8[:, k:k + 1], in1=rt_G, op0=ALU.is_equal, op1=ALU.mult,
                              accum_out=gate_all[:, gt, k:k + 1]), writes=R + [rsl])
                      S.op("dve", lambda e, gt=gt: e.tensor_scalar(out=slot_g_all[:, gt, :], in0=rt_s8[:, 0:4], scalar1=1.0,
                                                                   scalar2=-1.0, op0=ALU.max, op1=ALU.add),
                           writes=R + [rsl])
                      S.op("dve", lambda e: e.tensor_scalar(out=rt_small[:, 12:16], in0=rt_s8[:, 0:4], scalar1=0.5,
                                                            scalar2=None, op0=ALU.is_lt), writes=R)
                      S.op("dve", lambda e, gt=gt: e.scalar_tensor_tensor(
                          out=rt_small[:, 4:8], in0=trashc, scalar=float((gt % 8) * 512), in1=rt_small[:, 12:16],
                          op0=ALU.add, op1=ALU.mult), reads=[r_const], writes=R)
                      S.op("dve", lambda e: e.scalar_tensor_tensor(out=rt_small[:, 8:12], in0=rt_s8[:, 0:4], scalar=-1.0,
                                                                   in1=rt_small[:, 4:8], op0=ALU.add, op1=ALU.add), writes=R)
                      S.op("dve", lambda e, gt=gt: e.tensor_copy(out=slot_sc_all[:, gt, :], in_=rt_small[:, 8:12]),
                           writes=R + [rsl])
                      for k in range(4):
                          S.dma("pool", lambda e, k=k, gt=gt, mb_=mb_: e.indirect_dma_start(
                              out=Xs, out_offset=bass.IndirectOffsetOnAxis(ap=slot_sc_t[:, gt * 4 + k:gt * 4 + k + 1], axis=0),
                              in_=h2[mb_], in_offset=None),
                              reads=[rsl, r_h2[mb_]])

                  for k_ in range(TPS + 2):
                      if k_ < TPS:
                          stX(k_)
                      if 0 <= k_ - 1 < TPS:
                          stY(k_ - 1)
                      if 0 <= k_ - 2 < TPS and stage != "A":
                          stY2(k_ - 2)
              S.op("dve", lambda e, l=l: e.tensor_copy(out=dbg_sb[:, l * NE:(l + 1) * NE], in_=run_cnt),
                   reads=[r_run], writes=[r_dbg])
              S.barrier()
              AR.release(mA)
              if stage == "A":
                  continue

              mB = AR.mark()
              wgu = [AR.alloc([8, 2 * DFF], BF16) for _ in range(2)]
              wdn = [AR.alloc([4, D], BF16) for _ in range(2)]
              bdb = [AR.alloc([D]) for _ in range(2)]
              r_w = [Res(), Res()]
              xs = [AR.alloc([4, D], BF16) for _ in range(2)]
              r_xs = [Res(), Res()]
              XT = [AR.alloc([8, 512], BF16) for _ in range(2)]
              r_XT = [Res(), Res()]
              gcb = [AR.alloc([512]) for _ in range(2)]
              uab = [AR.alloc([512]) for _ in range(2)]
              sgb = [AR.alloc([512]) for _ in range(2)]
              ucb = [AR.alloc([512]) for _ in range(2)]
              tb = [AR.alloc([512]) for _ in range(2)]
              r_gc, r_ua, r_sg, r_uc, r_tb = ([Res(), Res()] for _ in range(5))
              actT = [AR.alloc([4, 512], BF16) for _ in range(2)]
              r_act = [Res(), Res()]
              ysbuf = [AR.alloc([4, D], BF16) for _ in range(2)]
              r_ys = [Res(), Res()]

              def load_w(ex, l=l):
                  wb = ex % 2
                  S.dma("pool", lambda e, l=l, ex=ex, wb=wb: e.dma_start(
                      out=wgu[wb], in_=w_gu[l, ex].rearrange("(c p) f -> p c f", p=128)), writes=[r_w[wb]])
                  S.dma("pool", lambda e, l=l, ex=ex, wb=wb: e.dma_start(
                      out=wdn[wb], in_=w_down[l, ex].rearrange("(c p) f -> p c f", p=128)), writes=[r_w[wb]])
                  S.dma("sp", lambda e, l=l, ex=ex, wb=wb: e.dma_start(
                      out=bdb[wb], in_=b_down[l, ex:ex + 1, :].partition_broadcast(128)), writes=[r_w[wb]])

              items = [(ex, g) for ex in range(NE) for g in range(NG)]

              def stT(i):
                  ex, g = items[i]
                  r0 = ex * C + g * 512
                  ib = i % 2
                  S.dma("sp", lambda e, r0=r0, ib=ib: e.dma_start(
                      out=xs[ib], in_=Xs[r0:r0 + 512, :].rearrange("(t p) d -> p t d", p=128)), writes=[r_xs[ib]])
                  for rt in range(4):
                      bank = 6 + (rt % 2)
                      pv = ps_bf(bank).rearrange("p (c t) -> p c t", c=8)
                      for c in range(8):
                          S.op("pe", lambda e, c=c, rt=rt, ib=ib, pv=pv: e.transpose(
                              out=pv[:, c, :], in_=xs[ib][:, rt, c * 128:(c + 1) * 128], identity=ident_b),
                              reads=[r_xs[ib], r_const], writes=[psr[bank]], signal=(c == 7))
                      if rt % 2 == 0:
                          S.op("act", lambda e, rt=rt, pv=pv, ib=ib: e.activation(
                              out=XT[ib][:, :, rt * 128:(rt + 1) * 128], in_=pv, func=AF.Identity),
                              reads=[psr[bank]], writes=[r_XT[ib]])
                      else:
                          S.op("dve", lambda e, rt=rt, pv=pv, ib=ib: e.tensor_copy(
                              out=XT[ib][:, :, rt * 128:(rt + 1) * 128], in_=pv),
                              reads=[psr[bank]], writes=[r_XT[ib]])

              def stG(i):
                  ex, g = items[i]
                  wb = ex % 2
                  ib = i % 2
                  for pr in range(4):
                      q2 = pr % 2
                      gbank = (pr % 2) * 2
                      ubank = gbank + 1
                      for (fo, bank) in ((pr, gbank), (pr + 4, ubank)):
                          for kc in range(8):
                              S.op("pe", lambda e, fo=fo, bank=bank, kc=kc, wb=wb, ib=ib: e.matmul(
                                  ps[bank][:], lhsT=wgu[wb][:, kc, fo * 128:(fo + 1) * 128], rhs=XT[ib][:, kc, :],
                                  start=(kc == 0), stop=(kc == 7)), reads=[r_w[wb], r_XT[ib]], writes=[psr[bank]],
                                  signal=(kc == 7))
                      S.op("act", lambda e, pr=pr, ex=ex, ubank=ubank, q2=q2: e.activation(
                          out=uab[q2], in_=ps[ubank][:], func=AF.Identity, bias=bguT[:, pr + 4, ex:ex + 1]),
                          reads=[psr[ubank], r_bgu], writes=[r_ua[q2]])
                      S.op("dve", lambda e, pr=pr, ex=ex, gbank=gbank, q2=q2: e.tensor_scalar(
                          out=gcb[q2], in0=ps[gbank][:], scalar1=bguT[:, pr, ex:ex + 1], scalar2=7.0, op0=ALU.add,
                          op1=ALU.min), reads=[psr[gbank], r_bgu], writes=[r_gc[q2]])
                      S.op("act", lambda e, q2=q2: e.activation(out=sgb[q2], in_=gcb[q2], func=AF.Sigmoid, scale=1.702),
                           reads=[r_gc[q2]], writes=[r_sg[q2]])
                      S.op("pool", lambda e, q2=q2: e.tensor_scalar(out=ucb[q2], in0=uab[q2], scalar1=7.0, scalar2=-7.0,
                                                                    op0=ALU.min, op1=ALU.max),
                           reads=[r_ua[q2]], writes=[r_uc[q2]])
                      S.op("dve", lambda e, q2=q2: e.tensor_tensor(out=tb[q2], in0=gcb[q2], in1=sgb[q2], op=ALU.mult),
                           reads=[r_gc[q2], r_sg[q2]], writes=[r_tb[q2]])
                      S.op("dve", lambda e, pr=pr, ib=ib, q2=q2: e.scalar_tensor_tensor(
                          out=actT[ib][:, pr, :], in0=ucb[q2], scalar=1.0, in1=tb[q2], op0=ALU.add, op1=ALU.mult),
                          reads=[r_uc[q2], r_tb[q2]], writes=[r_act[ib]])

              def stD(i):
                  ex, g = items[i]
                  r0 = ex * C + g * 512
                  wb = ex % 2
                  ib = i % 2
                  for rt in range(4):
                      for half in range(2):
                          bank = 4 + half
                          for fc in range(4):
                              S.op("pe", lambda e, rt=rt, half=half, fc=fc, ib=ib, wb=wb, bank=bank: e.matmul(
                                  ps[bank][:], lhsT=actT[ib][:, fc, rt * 128:(rt + 1) * 128],
                                  rhs=wdn[wb][:, fc, half * 512:(half + 1) * 512], start=(fc == 0), stop=(fc == 3)),
                                  reads=[r_act[ib], r_w[wb]], writes=[psr[bank]], signal=(fc == 3))
                          S.op("dve", lambda e, rt=rt, half=half, ib=ib, wb=wb, bank=bank: e.tensor_tensor(
                              out=ysbuf[ib][:, rt, half * 512:(half + 1) * 512], in0=ps[bank][:],
                              in1=bdb[wb][:, half * 512:(half + 1) * 512], op=ALU.add),
                              reads=[psr[bank], r_w[wb]], writes=[r_ys[ib]])
                  S.dma("sp", lambda e, r0=r0, ib=ib: e.dma_start(
                      out=Yd[r0:r0 + 512, :].rearrange("(t p) d -> p t d", p=128), in_=ysbuf[ib]), reads=[r_ys[ib]])

              load_w(0)
              stT(0)
              for i in range(len(items)):
                  ex, g = items[i]
                  if g == 0 and ex + 1 < NE:
                      load_w(ex + 1)
                  stG(i)
                  if i + 1 < len(items):
                      stT(i + 1)
                  stD(i)
              S.barrier()
              AR.release(mB)

              mC = AR.mark()
              DEPTH = 6
              g2b = [AR.alloc([D]) for _ in range(2)]
              r_g2 = [Res(), Res()]
              fgb = AR.alloc([D])
              r_fg = Res()
              xm = [AR.alloc([D]) for _ in range(DEPTH)]
              r_xm = [Res() for _ in range(DEPTH)]
              yk = [[AR.alloc([D], BF16) for _ in range(4)] for _ in range(DEPTH)]
              r_yk = [Res() for _ in range(DEPTH)]
              acc = [AR.alloc([D]) for _ in range(DEPTH)]
              r_acc = [Res() for _ in range(DEPTH)]
              sq2 = AR.alloc([D], BF16)
              r_sq2 = Res()
              st2 = AR.alloc([8])
              r_st2 = Res()
              if last_layer:
                  S.dma("sp", lambda e: e.dma_start(out=fgb, in_=final_g.partition_broadcast(128)), writes=[r_fg])

              def cL(gt):
                  s_ = gt // TPS
                  b = gt % DEPTH
                  if gt % TPS == 0:
                      S.dma("sp", lambda e, s_=s_: e.dma_start(out=g2b[s_ % 2],
                                                                in_=modscr[s_, 6:7, :].partition_broadcast(128)),
                            writes=[r_g2[s_ % 2]])
                  S.dma("sp", lambda e, gt=gt, b=b: e.dma_start(out=xm[b], in_=xres[gt * 128:(gt + 1) * 128, :]),
                        writes=[r_xm[b]])
                  for k in range(4):
                      S.dma("pool", lambda e, gt=gt, b=b, k=k: e.indirect_dma_start(
                          out=yk[b][k], out_offset=None, in_=Yd,
                          in_offset=bass.IndirectOffsetOnAxis(ap=slot_g_t[:, gt * 4 + k:gt * 4 + k + 1], axis=0)),
                          reads=[r_slot_t[gt]], writes=[r_yk[b]])

              def cC(gt):
                  s_ = gt // TPS
                  b = gt % DEPTH
                  gb = g2b[s_ % 2]
                  S.op("dve", lambda e, gt=gt, b=b: e.tensor_scalar(out=acc[b], in0=yk[b][0], scalar1=gate_all[:, gt, 0:1],
                                                                    scalar2=None, op0=ALU.mult),
                       reads=[r_yk[b], r_slot_t[gt]], writes=[r_acc[b]])
                  for k in range(1, 4):
                      S.op("dve", lambda e, gt=gt, b=b, k=k: e.scalar_tensor_tensor(
                          out=acc[b], in0=yk[b][k], scalar=gate_all[:, gt, k:k + 1], in1=acc[b], op0=ALU.mult, op1=ALU.add),
                          reads=[r_yk[b], r_slot_t[gt]], writes=[r_acc[b]])
                  S.op("dve", lambda e, b=b, gb=gb: e.tensor_tensor(out=acc[b], in0=acc[b], in1=gb, op=ALU.mult),
                       reads=[r_g2[s_ % 2]], writes=[r_acc[b]])
                  S.op("dve", lambda e, b=b: e.tensor_tensor(out=xm[b], in0=acc[b], in1=xm[b], op=ALU.add),
                       reads=[r_acc[b]], writes=[r_xm[b]])
                  if last_layer:
                      S.op("act", lambda e, b=b: e.activation(out=sq2, in_=xm[b], func=AF.Square, accum_out=st2[:, 0:1]),
                           reads=[r_xm[b]], writes=[r_sq2, r_st2])
                      S.op("act", lambda e: e.activation(out=st2[:, 1:2], in_=st2[:, 0:1], func=AF.Sqrt, scale=1.0 / D,
                                                         bias=EPS), reads=[r_st2], writes=[r_st2])
                      S.op("dve", lambda e: e.reciprocal(out=st2[:, 2:3], in_=st2[:, 1:2]), reads=[r_st2], writes=[r_st2])
                      S.op("dve", lambda e, b=b: e.scalar_tensor_tensor(out=xm[b], in0=xm[b], scalar=st2[:, 2:3],
                                                                        in1=fgb, op0=ALU.mult, op1=ALU.mult),
                           reads=[r_st2, r_fg], writes=[r_xm[b]])
                  S.dma("sp", lambda e, gt=gt, b=b: e.dma_start(out=y[gt * 128:(gt + 1) * 128, :], in_=xm[b]),
                        reads=[r_xm[b]])

              LA = DEPTH - 1
              for i_ in range(NT + LA):
                  if i_ < NT:
                      cL(i_)
                  if i_ >= LA:
                      cC(i_ - LA)
              S.barrier()
              AR.release(mC)

          except _Stop:
            S.barrier()
            break
        if stage == "full":
            S.dma("sp", lambda e: e.dma_start(out=dbg, in_=dbg_sb), reads=[r_dbg])
        S.final_wait("sp")
        S.emit()
        print("instructions:", S.n_inst, flush=True)
    return nc


def make_consts():
    bf = ml_dtypes.bfloat16
    s = np.arange(SEQ, dtype=np.int64)
    ph = (np.outer(s, s) % SEQ).astype(np.float64) * (2.0 * np.pi / SEQ)
    def blk(m):
        m = m.astype(np.float32).reshape(TPS, 128, TPS, 128).transpose(2, 1, 0, 3)
        return np.ascontiguousarray(m.reshape(TPS, 128, TPS * 128)).astype(bf)
    dftc = blk(np.cos(ph))
    dfts = blk(np.sin(ph))
    c = np.arange(64, dtype=np.int64)
    ph64 = (np.outer(c, c) % 64).astype(np.float64) * (2.0 * np.pi / 64)
    nrm = 1.0 / np.sqrt(SEQ * 64.0)
    cc = np.cos(ph64) * nrm
    sc = np.sin(ph64) * nrm
    ccb = np.zeros((128, 128), np.float32)
    scb = np.zeros((128, 128), np.float32)
    for g in range(2):
        ccb[g * 64:(g + 1) * 64, g * 64:(g + 1) * 64] = cc
        scb[g * 64:(g + 1) * 64, g * 64:(g + 1) * 64] = sc
    inv_freq = np.power(np.float32(500000.0), -np.arange(0, 16, 2, dtype=np.float32) / np.float32(16))
    ang = np.arange(SEQ, dtype=np.float32)[:, None] * inv_freq[None, :]
    a = np.arange(128)
    consts = {
        "dftc": dftc, "dfts": dfts, "ccb": ccb, "scb": scb,
        "rope_c": np.cos(ang).astype(np.float32), "rope_s": np.sin(ang).astype(np.float32),
        "k_ident": np.eye(128, dtype=np.float32),
        "k_lstrict": (a[:, None] < a[None, :]).astype(np.float32),
        "k_maska": (a[None, :] <= a[:, None]).astype(np.float32),
        "k_maskb": (a[:, None] <= a[None, :]).astype(np.float32),
    }
    return consts


_NC_CACHE = {}


def run(x_all, c_all, weights, NL, NSEQ, C, n_cores, stage="full"):
    key = (NL, NSEQ, C, stage)
    if key not in _NC_CACHE:
        _NC_CACHE[key] = build(NL, NSEQ, C, stage)
    nc = _NC_CACHE[key]
    consts = make_consts()
    consts["k_ebase"] = np.tile((np.arange(NE, dtype=np.float32) * C)[None, :], (128, 1)).astype(np.float32)
    consts["k_trash"] = (NE * C + 1 + np.arange(128, dtype=np.float32)[:, None]
                         + 128.0 * np.arange(4, dtype=np.float32)[None, :]).astype(np.float32)
    shared = dict(consts)
    for k, v in weights.items():
        v = np.ascontiguousarray(np.asarray(v, dtype=np.float32))
        if k == "final_g":
            v = v.reshape(1, D)
        else:
            v = v[:NL]
        shared[k] = v
    in_maps = []
    for ci in range(n_cores):
        m = dict(shared)
        xs = x_all[ci * NSEQ:(ci + 1) * NSEQ]
        m["x_in"] = np.ascontiguousarray(xs.reshape(NSEQ * SEQ, D))
        m["cT"] = np.ascontiguousarray(c_all[ci * NSEQ:(ci + 1) * NSEQ].T)
        in_maps.append(m)
    res = run_bass_kernel_spmd(nc, in_maps, core_ids=list(range(n_cores)))
    outs = [np.asarray(r["y"]).reshape(NSEQ, SEQ, D) for r in res.results]
    if stage == "full":
        try:
            cnts = np.stack([np.asarray(r["dbg"])[0].reshape(NL, NE) for r in res.results])
            print("expert-count max per layer (capacity %d):" % C, cnts.max(axis=(0, 2)).tolist(),
                  "overflow assignments per layer:", np.maximum(cnts - C, 0).sum(axis=(0, 2)).tolist(), flush=True)
        except Exception as ex:
            print("dbg unavailable", ex)
    return np.concatenate(outs, axis=0)


CAPACITY = 3072


def kernel(x_prompt, x_sample, c_prompt, c_sample, norm1_g, ada_w, ada_b, w_in, b_in, w_fmix, sinks, w_o, b_o,
           norm2_g, w_router, b_router, w_gu, b_gu, w_down, b_down, final_g):
    x_prompt = np.asarray(x_prompt, dtype=np.float32)
    x_sample = np.asarray(x_sample, dtype=np.float32)
    c_prompt = np.asarray(c_prompt, dtype=np.float32)
    c_sample = np.asarray(c_sample, dtype=np.float32)
    n_cores = 8
    pp = x_prompt.shape[0] // n_cores
    sp = x_sample.shape[0] // n_cores
    xs, cs = [], []
    for ci in range(n_cores):
        xs.append(x_prompt[ci * pp:(ci + 1) * pp])
        xs.append(x_sample[ci * sp:(ci + 1) * sp])
        cs.append(c_prompt[ci * pp:(ci + 1) * pp])
        cs.append(c_sample[ci * sp:(ci + 1) * sp])
    x_all = np.concatenate(xs, axis=0)
    c_all = np.concatenate(cs, axis=0)
    weights = dict(norm1_g=norm1_g, ada_w=ada_w, ada_b=ada_b, w_in=w_in, b_in=b_in, w_fmix=w_fmix, sinks=sinks,
                   w_o=w_o, b_o=b_o, norm2_g=norm2_g, w_router=w_router, b_router=b_router, w_gu=w_gu, b_gu=b_gu,
                   w_down=w_down, b_down=b_down, final_g=final_g)
    out = run(x_all, c_all, weights, 4, pp + sp, CAPACITY, n_cores)
    nseq = pp + sp
    yp = np.concatenate([out[ci * nseq:ci * nseq + pp] for ci in range(n_cores)], axis=0)
    ys = np.concatenate([out[ci * nseq + pp:(ci + 1) * nseq] for ci in range(n_cores)], axis=0)
    return (np.ascontiguousarray(yp, dtype=np.float32), np.ascontiguousarray(ys, dtype=np.float32))
```

```python
import contextlib
import numpy as np
import ml_dtypes
import concourse.bass as bass
import concourse.mybir as mybir
from concourse.bass_utils import run_bass_kernel_spmd

F32 = mybir.dt.float32
BF16 = mybir.dt.bfloat16
I32 = mybir.dt.int32
AF = mybir.ActivationFunctionType
ALU = mybir.AluOpType

SAME_ENGINE_SYNC = True

D = 1024
SEQ = 2048
TPS = 16
NE = 32
DFF = 512
INW = 1536
EPS = 1e-5


class _Stop(Exception):
    pass


class Res:
    __slots__ = ("name", "w", "r", "excl")

    def __init__(self, name="", excl=False):
        self.name = name
        self.w = None
        self.r = []
        self.excl = excl


class Sched:
    ENGS = ("pe", "act", "dve", "pool", "sp")

    def __init__(self, nc, stack, n_dma_sems=48):
        self.nc = nc
        self.streams = {e: [] for e in self.ENGS}
        self.count = {e: 0 for e in self.ENGS}
        self.pending = {e: False for e in self.ENGS}
        self.waited = {e: {} for e in self.ENGS}
        self.sems = {}
        for e in self.ENGS:
            self.sems["E_" + e] = stack.enter_context(nc.semaphore("sem_" + e))
        self.dma_sems = []
        for i in range(n_dma_sems):
            k = "D_%d" % i
            self.sems[k] = stack.enter_context(nc.semaphore("semd_%d" % i))
            self.dma_sems.append(k)
        self.dma_val = {k: 0 for k in self.dma_sems}
        self.dma_rr = 0
        self.dma_rr_sw = 0
        self.n_inst = 0

    def _need(self, eng, tok):
        if tok is None:
            return
        key, val, teng = tok
        if teng == eng:
            if eng == "pe" or not SAME_ENGINE_SYNC or val > self.count[eng]:
                return
        if self.waited[eng].get(key, 0) >= val:
            return
        self.waited[eng][key] = val
        self.streams[eng].append(("wait", key, val))

    def _deps(self, eng, reads, writes):
        for r in reads:
            self._need(eng, r.w)
        for w in writes:
            self._need(eng, w.w)
            for t in w.r:
                self._need(eng, t)

    def _commit(self, tok, reads, writes):
        for r in reads:
            r.r.append(tok)
            if len(r.r) > 48:
                best = {}
                for t in r.r:
                    if t[0] not in best or best[t[0]][1] < t[1]:
                        best[t[0]] = t
                r.r = list(best.values())
        for w in writes:
            w.w = tok
            w.r = []

    def op(self, eng, fn, reads=(), writes=(), signal=True):
        if any(r.excl for r in reads):
            writes = list(writes) + [r for r in reads if r.excl]
            reads = [r for r in reads if not r.excl]
        self._deps(eng, reads, writes)
        if signal:
            self.count[eng] += 1
            val = self.count[eng]
            self.streams[eng].append(("inst", fn, "E_" + eng))
            self.pending[eng] = False
        else:
            val = self.count[eng] + 1
            self.streams[eng].append(("inst", fn, None))
            self.pending[eng] = True
        tok = ("E_" + eng, val, eng)
        self._commit(tok, reads, writes)
        self.n_inst += 1
        return tok

    def dma(self, eng, fn, reads=(), writes=()):
        self._deps(eng, reads, writes)
        half = len(self.dma_sems) // 2
        if eng == "pool":
            key = self.dma_sems[self.dma_rr_sw % half]
            self.dma_rr_sw += 1
        else:
            key = self.dma_sems[half + self.dma_rr % half]
            self.dma_rr += 1
        prev = self.dma_val[key]
        if prev > 0:
            self._need(eng, (key, prev, None))
        self.dma_val[key] = prev + 16
        self.streams[eng].append(("dma", fn, key))
        tok = (key, prev + 16, None)
        self._commit(tok, reads, writes)
        self.n_inst += 1
        return tok

    def barrier(self):
        toks = []
        for e in self.ENGS:
            if self.pending[e]:
                raise RuntimeError("pending unsignalled instrs on " + e)
            if self.count[e] > 0:
                toks.append(("E_" + e, self.count[e], e))
        for k in self.dma_sems:
            if self.dma_val[k] > 0:
                toks.append((k, self.dma_val[k], None))
        for e in self.ENGS:
            for t in toks:
                if t[2] == e:
                    continue
                self._need(e, t)

    def final_wait(self, eng="sp"):
        for k in self.dma_sems:
            if self.dma_val[k] > 0:
                self._need(eng, (k, self.dma_val[k], None))
        for e in self.ENGS:
            if e != eng and self.count[e] > 0:
                self._need(eng, ("E_" + e, self.count[e], e))

    def emit(self):
        nc = self.nc
        sems = self.sems
        streams = self.streams

        def run(engobj, items):
            for it in items:
                if it[0] == "wait":
                    engobj.wait_ge(sems[it[1]], it[2])
                elif it[0] == "inst":
                    ins = it[1](engobj)
                    if it[2] is not None:
                        ins.then_inc(sems[it[2]], 1)
                else:
                    ins = it[1](engobj)
                    ins.then_inc(sems[it[2]], 16)

        with nc.Block() as block:
            @block.tensor
            def _(e):
                run(e, streams["pe"])

            @block.scalar
            def _(e):
                run(e, streams["act"])

            @block.vector
            def _(e):
                run(e, streams["dve"])

            @block.gpsimd
            def _(e):
                run(e, streams["pool"])

            @block.sync
            def _(e):
                run(e, streams["sp"])


class Arena:
    def __init__(self, ap_f32, nwords):
        self.ap = ap_f32
        self.n = nwords
        self.top = 0

    def mark(self):
        return self.top

    def release(self, m):
        self.top = m

    def alloc(self, free_shape, dtype=F32, parts=128):
        nel = 1
        for s in free_shape:
            nel *= s
        esz = 4 if dtype in (F32, I32) else 2
        nw = (nel * esz + 3) // 4
        nw = (nw + 7) // 8 * 8
        if self.top + nw > self.n:
            raise RuntimeError("SBUF arena overflow: need %d words, top %d, cap %d" % (nw, self.top, self.n))
        a = self.ap[0:parts, self.top:self.top + nw]
        self.top += nw
        if dtype != F32:
            a = a.bitcast(dtype)
        a = a[:, 0:nel]
        if len(free_shape) == 2:
            a = a.rearrange("p (a b) -> p a b", a=free_shape[0])
        elif len(free_shape) == 3:
            a = a.rearrange("p (a b c) -> p a b c", a=free_shape[0], b=free_shape[1])
        return a


Q_PAIRS = [(0, 3), (1, 4), (2, 5), (6, 9), (7, 10), (8, 11)]


def build(NL, NSEQ, C, stage="full"):
    T = NSEQ * SEQ
    NT = T // 128
    NG = C // 512
    nc = bass.Bass("TRN2", target_bir_lowering=False)

    def din(name, shape, dt=F32):
        return nc.dram_tensor(name, list(shape), dt, kind="ExternalInput").ap()

    x_in = din("x_in", [T, D])
    cT = din("cT", [D, NSEQ])
    norm1_g = din("norm1_g", [NL, D])
    ada_w = din("ada_w", [NL, D, 6 * D])
    ada_b = din("ada_b", [NL, 6 * D])
    w_in = din("w_in", [NL, D, INW])
    b_in = din("b_in", [NL, INW])
    w_fmix = din("w_fmix", [NL, 4, 64, 64])
    sinks = din("sinks", [NL, 12])
    w_o = din("w_o", [NL, D, D])
    b_o = din("b_o", [NL, D])
    norm2_g = din("norm2_g", [NL, D])
    w_router = din("w_router", [NL, D, NE])
    b_router = din("b_router", [NL, NE])
    w_gu = din("w_gu", [NL, NE, D, 2 * DFF])
    b_gu = din("b_gu", [NL, NE, 2 * DFF])
    w_down = din("w_down", [NL, NE, DFF, D])
    b_down = din("b_down", [NL, NE, D])
    final_g = din("final_g", [1, D])
    dftc = din("dftc", [TPS, 128, TPS * 128], BF16)
    dfts = din("dfts", [TPS, 128, TPS * 128], BF16)
    ccb = din("ccb", [128, 128])
    scb = din("scb", [128, 128])
    rope_c = din("rope_c", [SEQ, 8])
    rope_s = din("rope_s", [SEQ, 8])
    k_ident = din("k_ident", [128, 128])
    k_lstrict = din("k_lstrict", [128, 128])
    k_maska = din("k_maska", [128, 128])
    k_maskb = din("k_maskb", [128, 128])
    k_ebase = din("k_ebase", [128, NE])
    k_trash = din("k_trash", [128, 4])

    y = nc.dram_tensor("y", [T, D], F32, kind="ExternalOutput").ap()
    dbg = nc.dram_tensor("dbg", [128, NL * NE], F32, kind="ExternalOutput").ap()
    xres = nc.dram_tensor("xres", [T, D], F32, kind="Internal").ap()
    Xs = nc.dram_tensor("Xs", [NE * C + 4096, D], BF16, kind="Internal").ap()
    Yd = nc.dram_tensor("Yd", [NE * C, D], BF16, kind="Internal").ap()
    modscr = nc.dram_tensor("modscr", [NSEQ, 7, D], F32, kind="Internal").ap()

    with contextlib.ExitStack() as st:
        S = Sched(nc, st)
        ARENA_WORDS = 53120 - 2 * NT * 4 - 16
        arena_t = st.enter_context(nc.sbuf_tensor("arena", [128, ARENA_WORDS], F32))
        AR = Arena(arena_t, ARENA_WORDS)
        ps = [st.enter_context(nc.psum_tensor("psb%d" % i, [128, 512], F32)) for i in range(8)]
        psr = [Res("ps%d" % i, excl=True) for i in range(8)]

        def ps_bf(i):
            return ps[i][:].bitcast(BF16)

        ident_f = AR.alloc([128])
        ident_b = AR.alloc([128], BF16)
        lstrict_b = AR.alloc([128], BF16)
        ones_b = AR.alloc([128], BF16)
        maska_b = AR.alloc([128], BF16)
        maskb_b = AR.alloc([128], BF16)
        ebase = AR.alloc([NE])
        trashc = AR.alloc([4])
        run_cnt = AR.alloc([NE])
        dbg_sb = AR.alloc([NL * NE])
        r_dbg = Res()
        ropec = AR.alloc([TPS, 8])
        ropes = AR.alloc([TPS, 8])
        tmpc = AR.alloc([128])
        slot_sc_t = st.enter_context(nc.sbuf_tensor("slot_sc", [128, NT * 4], I32))
        slot_g_t = st.enter_context(nc.sbuf_tensor("slot_g", [128, NT * 4], I32))
        slot_sc_all = slot_sc_t[:, :].rearrange("p (t k) -> p t k", k=4)
        slot_g_all = slot_g_t[:, :].rearrange("p (t k) -> p t k", k=4)
        gate_all = AR.alloc([NT, 4])
        siluT = AR.alloc([8, NSEQ])
        siluT_r = Res()
        r_const = Res("const")
        r_run = Res("run")
        r_slots = Res("slots")
        r_slot_t = [Res() for _ in range(NT)]

        def ld(dst, src, res, eng="sp"):
            S.dma(eng, lambda e: e.dma_start(out=dst, in_=src), writes=[res])

        ld(ident_f, k_ident, r_const)
        S.op("dve", lambda e: e.tensor_copy(out=ident_b, in_=ident_f), reads=[r_const], writes=[r_const])
        rr = Res()
        for (dst, src) in ((lstrict_b, k_lstrict), (maska_b, k_maska), (maskb_b, k_maskb)):
            ld(tmpc, src, rr)
            S.op("dve", lambda e, dst=dst: e.tensor_copy(out=dst, in_=tmpc), reads=[rr], writes=[r_const, rr])
        S.op("pool", lambda e: e.memset(ones_b, 1.0), writes=[r_const])
        ld(ebase, k_ebase, r_const)
        ld(trashc, k_trash, r_const)
        ld(ropec, rope_c.rearrange("(t p) i -> p t i", p=128), r_const)
        ld(ropes, rope_s.rearrange("(t p) i -> p t i", p=128), r_const)
        S.dma("sp", lambda e: e.dma_start(out=siluT, in_=cT.rearrange("(c p) s -> p c s", p=128),
                                          allow_slow_non_contiguous=True), writes=[siluT_r])
        S.op("act", lambda e: e.activation(out=siluT, in_=siluT, func=AF.Silu), reads=[siluT_r], writes=[siluT_r])

        w_in_sb = AR.alloc([8, INW], BF16)
        w_o_sb = AR.alloc([8, D], BF16)
        wr_sb = AR.alloc([8, NE], BF16)
        brow = AR.alloc([INW + NE], BF16, parts=1)
        ablk = AR.alloc([2, 128], BF16)
        nbblk = AR.alloc([2, 128], BF16)
        bguT = AR.alloc([8, NE])
        expsink = AR.alloc([12])
        r_win, r_wo, r_wr, r_brow, r_ablk, r_bgu, r_sink = (Res() for _ in range(7))

        phase_mark = AR.mark()
        if stage not in ("A", "A0", "A1", "A3"):
            zt = AR.alloc([4, D], BF16)
            r_zt = Res()
            S.op("pool", lambda e: e.memset(zt, 0.0), writes=[r_zt])
            for r0 in range(0, NE * C, 512):
                S.dma("sp", lambda e, r0=r0: e.dma_start(out=Xs[r0:r0 + 512, :].rearrange("(t p) d -> p t d", p=128),
                                                        in_=zt), reads=[r_zt])
            for r0 in range(NE * C, NE * C + 4096, 512):
                S.dma("sp", lambda e, r0=r0: e.dma_start(out=Xs[r0:r0 + 512, :].rearrange("(t p) d -> p t d", p=128),
                                                        in_=zt), reads=[r_zt])
            S.barrier()
            AR.release(phase_mark)

        def xsrc(l):
            return x_in if l == 0 else y

        for l in range(NL):
          try:
              last_layer = (l == NL - 1)
              S.dma("pool", lambda e, l=l: e.dma_start(out=w_in_sb, in_=w_in[l].rearrange("(c p) f -> p c f", p=128)),
                    writes=[r_win])
              S.dma("pool", lambda e, l=l: e.dma_start(out=w_o_sb, in_=w_o[l].rearrange("(c p) f -> p c f", p=128)),
                    writes=[r_wo])
              S.dma("pool", lambda e, l=l: e.dma_start(out=wr_sb, in_=w_router[l].rearrange("(c p) f -> p c f", p=128)),
                    writes=[r_wr])
              S.dma("pool", lambda e, l=l: e.dma_start(out=brow[:, 0:INW], in_=b_in[l:l + 1, :]), writes=[r_brow])
              S.dma("pool", lambda e, l=l: e.dma_start(out=brow[:, INW:INW + NE], in_=b_router[l:l + 1, :]),
                    writes=[r_brow])

              m0 = AR.mark()
              adaw = [AR.alloc([8, 512]) for _ in range(2)]
              r_adaw = [Res(), Res()]
              modblk = [AR.alloc([512], parts=NSEQ) for _ in range(2)]
              r_modblk = [Res(), Res()]
              gainb = AR.alloc([3, D], parts=NSEQ)
              adab = AR.alloc([6 * D], parts=NSEQ)
              r_gain = Res()
              g1keep = AR.alloc([D], parts=NSEQ)
              r_g1keep = Res()
              for i, src in enumerate((norm1_g, norm2_g, b_o)):
                  S.dma("sp", lambda e, i=i, src=src, l=l: e.dma_start(
                      out=gainb[:, i, :], in_=src[l:l + 1, :].partition_broadcast(NSEQ)), writes=[r_gain])
              S.dma("sp", lambda e, l=l: e.dma_start(out=adab, in_=ada_b[l:l + 1, :].partition_broadcast(NSEQ)),
                    writes=[r_gain])
              for cb in range(12):
                  b = cb % 2
                  m = cb // 2
                  hcol = (cb % 2) * 512
                  S.dma("sp", lambda e, l=l, cb=cb, b=b: e.dma_start(
                      out=adaw[b], in_=ada_w[l, :, cb * 512:(cb + 1) * 512].rearrange("(c p) f -> p c f", p=128)),
                      writes=[r_adaw[b]])
                  for kc in range(8):
                      S.op("pe", lambda e, kc=kc, b=b: e.matmul(ps[0][0:NSEQ, :], lhsT=siluT[:, kc, :], rhs=adaw[b][:, kc, :],
                                                               start=(kc == 0), stop=(kc == 7)),
                           reads=[siluT_r, r_adaw[b]], writes=[psr[0]], signal=(kc == 7))
                  mb = modblk[b]
                  S.op("dve", lambda e, mb=mb, cb=cb: e.tensor_tensor(out=mb, in0=ps[0][0:NSEQ, :],
                                                                       in1=adab[:, cb * 512:(cb + 1) * 512], op=ALU.add),
                       reads=[psr[0], r_gain], writes=[r_modblk[b]])
                  if m in (1, 4):
                      gi = 0 if m == 1 else 1
                      S.op("dve", lambda e, mb=mb, gi=gi, hcol=hcol: e.scalar_tensor_tensor(
                          out=mb, in0=mb, scalar=1.0, in1=gainb[:, gi, hcol:hcol + 512], op0=ALU.add, op1=ALU.mult),
                          reads=[r_gain], writes=[r_modblk[b]])
                  dslot = {0: 1, 1: 0, 2: 2, 3: 5, 4: 4, 5: 6}[m]
                  S.dma("sp", lambda e, mb=mb, dslot=dslot, hcol=hcol: e.dma_start(
                      out=modscr[:, dslot, hcol:hcol + 512], in_=mb), reads=[r_modblk[b]])
                  if m == 2:
                      S.op("dve", lambda e, mb=mb, hcol=hcol: e.tensor_tensor(
                          out=g1keep[:, hcol:hcol + 512], in0=mb, in1=gainb[:, 2, hcol:hcol + 512], op=ALU.mult),
                          reads=[r_modblk[b], r_gain], writes=[r_g1keep])
              S.dma("sp", lambda e: e.dma_start(out=modscr[:, 3, :], in_=g1keep), reads=[r_g1keep])

              ccs = AR.alloc([2, 128])
              wblk = AR.alloc([2, 128])
              r_ccs, r_wblk = Res(), Res()
              ld(ccs[:, 0, :], ccb, r_ccs)
              ld(ccs[:, 1, :], scb, r_ccs)
              S.op("pool", lambda e: e.memset(wblk, 0.0), writes=[r_wblk])
              for cc in range(2):
                  for gg in range(2):
                      S.dma("sp", lambda e, l=l, cc=cc, gg=gg: e.dma_start(
                          out=wblk[gg * 64:(gg + 1) * 64, cc, gg * 64:(gg + 1) * 64], in_=w_fmix[l, 2 * cc + gg]),
                          writes=[r_wblk])
              for cc in range(2):
                  for t in range(2):
                      S.op("pe", lambda e, cc=cc, t=t: e.matmul(ps[1][:, t * 256 + cc * 128:t * 256 + (cc + 1) * 128],
                                                               lhsT=ccs[:, t, :], rhs=wblk[:, cc, :], start=True, stop=True),
                           reads=[r_ccs, r_wblk], writes=[psr[1]])
              S.op("act", lambda e: e.activation(out=ablk, in_=ps[1][:, 0:256].rearrange("p (a b) -> p a b", a=2),
                                                 func=AF.Identity), reads=[psr[1]], writes=[r_ablk])
              S.op("act", lambda e: e.activation(out=nbblk, in_=ps[1][:, 256:512].rearrange("p (a b) -> p a b", a=2),
                                                 func=AF.Identity, scale=-1.0), reads=[psr[1]], writes=[r_ablk])
              bgr = AR.alloc([2 * DFF], parts=NE)
              r_bgr = Res()
              ld(bgr, b_gu[l], r_bgr)
              for c8 in range(8):
                  S.op("pe", lambda e, c8=c8: e.transpose(out=ps[2][:, c8 * NE:(c8 + 1) * NE],
                                                          in_=bgr[:, c8 * 128:(c8 + 1) * 128], identity=ident_f[0:NE, 0:NE]),
                       reads=[r_bgr, r_const], writes=[psr[2]], signal=(c8 == 7))
              S.op("act", lambda e: e.activation(out=bguT, in_=ps[2][:, 0:8 * NE].rearrange("p (a b) -> p a b", a=8),
                                                 func=AF.Identity), reads=[psr[2]], writes=[r_bgu])
              S.dma("sp", lambda e, l=l: e.dma_start(out=expsink, in_=sinks[l:l + 1, :].partition_broadcast(128)),
                    writes=[r_sink])
              S.op("act", lambda e: e.activation(out=expsink, in_=expsink, func=AF.Exp), reads=[r_sink], writes=[r_sink])
              S.op("pool", lambda e: e.memset(run_cnt, 0.0), writes=[r_run])
              S.barrier()
              AR.release(m0)
              if stage == 'A0':
                  raise _Stop()

              mA = AR.mark()
              bcf = AR.alloc([6, D])
              r_bc = Res()
              xt = [AR.alloc([D]) for _ in range(2)]
              r_xt = [Res(), Res()]
              tmpf = AR.alloc([D])
              r_tmpf = Res()
              sq = tmpf
              r_sq = r_tmpf
              hb = [AR.alloc([D], BF16) for _ in range(2)]
              r_hb = [Res(), Res()]
              hT = [AR.alloc([8, 128], BF16) for _ in range(2)]
              r_hT = [Res(), Res()]
              stat = AR.alloc([16])
              r_stat = Res()
              f_tm = AR.alloc([TPS, 256], BF16)
              r_f = Res()
              v_aug = AR.alloc([TPS, 4, 66], BF16)
              r_v = Res()
              qk_tm = [AR.alloc([1024], BF16) for _ in range(2)]
              r_qk = [Res(), Res()]
              ropet = AR.alloc([4, 16, 8])
              r_ropet = Res()
              QT = AR.alloc([6, SEQ], BF16)
              KT = AR.alloc([2, SEQ], BF16)
              r_QT, r_KT = Res(), Res()
              dftb = [[AR.alloc([TPS, 128], BF16) for _ in range(2)] for _ in range(2)]
              r_dft = [Res(), Res()]
              ysb = [AR.alloc([2, 2, 128], BF16) for _ in range(2)]
              r_ysb = [Res(), Res()]
              foT = AR.alloc([2, SEQ], BF16)
              r_foT = Res()
              PT = [AR.alloc([3, 128], BF16) for _ in range(6)]
              r_PT = [Res() for _ in range(6)]
              den = AR.alloc([12])
              r_den = Res()
              attn_tm = [AR.alloc([12, 64], BF16) for _ in range(2)]
              r_attn = [Res(), Res()]
              attnT = [AR.alloc([6, 128], BF16) for _ in range(2)]
              r_attnT = [Res(), Res()]
              xb = AR.alloc([D])
              r_xb = Res()
              xmid_ = AR.alloc([D])
              xmid = [xmid_, xmid_]
              r_xmid_ = Res()
              r_xmid = [r_xmid_, r_xmid_]
              h2 = [AR.alloc([D], BF16) for _ in range(2)]
              r_h2 = [Res(), Res()]
              h2T = AR.alloc([8, 128], BF16)
              r_h2T = Res()
              rt_lg = AR.alloc([NE])
              rt_t8 = AR.alloc([8])
              rt_mask = AR.alloc([NE])
              rt_maskb = AR.alloc([NE], BF16)
              rt_e = AR.alloc([NE])
              rt_G = AR.alloc([NE])
              rt_cnt = AR.alloc([NE])
              rt_ms = AR.alloc([NE])
              rt_s8 = AR.alloc([8])
              rt_small = AR.alloc([16])
              rt_junk = AR.alloc([NE])
              r_rt = Res()
              S.op("pool", lambda e: e.memset(v_aug, 1.0), writes=[r_v])

              def rms_mod(xtile, r_x, a_idx, s_idx, out_b, r_out):
                  S.op("act", lambda e: e.activation(out=sq, in_=xtile, func=AF.Square, accum_out=stat[:, 0:1]),
                       reads=[r_x], writes=[r_sq, r_stat])
                  S.op("act", lambda e: e.activation(out=stat[:, 1:2], in_=stat[:, 0:1], func=AF.Sqrt,
                                                     scale=1.0 / D, bias=EPS), reads=[r_stat], writes=[r_stat])
                  S.op("dve", lambda e: e.reciprocal(out=stat[:, 2:3], in_=stat[:, 1:2]), reads=[r_stat], writes=[r_stat])
                  S.op("dve", lambda e: e.scalar_tensor_tensor(out=tmpf, in0=xtile, scalar=stat[:, 2:3],
                                                               in1=bcf[:, a_idx, :], op0=ALU.mult, op1=ALU.mult),
                       reads=[r_x, r_stat, r_bc], writes=[r_tmpf])
                  S.op("dve", lambda e: e.tensor_tensor(out=out_b, in0=tmpf, in1=bcf[:, s_idx, :], op=ALU.add),
                       reads=[r_tmpf, r_bc], writes=[r_out])

              def transpose8(src_b, r_src, bank, dst, r_dst, evac_eng):
                  pv = ps_bf(bank).rearrange("p (c t) -> p c t", c=8)
                  for c in range(8):
                      S.op("pe", lambda e, c=c: e.transpose(out=pv[:, c, :], in_=src_b[:, c * 128:(c + 1) * 128],
                                                            identity=ident_b), reads=[r_src, r_const],
                           writes=[psr[bank]], signal=(c == 7))
                  if evac_eng == "act":
                      S.op("act", lambda e: e.activation(out=dst, in_=pv, func=AF.Identity),
                           reads=[psr[bank]], writes=[r_dst])
                  else:
                      S.op("dve", lambda e: e.tensor_copy(out=dst, in_=pv), reads=[psr[bank]], writes=[r_dst])

              for s in range(NSEQ):
                  S.dma("sp", lambda e, s=s: e.dma_start(
                      out=bcf, in_=modscr[s:s + 1, 0:6, :].rearrange("o a d -> o (a d)").partition_broadcast(128)),
                      writes=[r_bc])
                  def stA(tt, s=s, l=l):
                      gt = s * TPS + tt
                      b = tt % 2
                      S.dma("sp", lambda e, gt=gt, b=b, l=l: e.dma_start(out=xt[b], in_=xsrc(l)[gt * 128:(gt + 1) * 128, :]),
                            writes=[r_xt[b]])
                      rms_mod(xt[b], r_xt[b], 0, 1, hb[b], r_hb[b])

                  def stA2(tt, s=s, l=l):
                      b = tt % 2
                      transpose8(hb[b], r_hb[b], 7, hT[b], r_hT[b], "act")

                  def stB(tt, s=s, l=l):
                      gt = s * TPS + tt
                      b = tt % 2
                      for cb in range(3):
                          for kc in range(8):
                              S.op("pe", lambda e, cb=cb, kc=kc, b=b: e.matmul(
                                  ps[cb][:], lhsT=hT[b][:, kc, :], rhs=w_in_sb[:, kc, cb * 512:(cb + 1) * 512],
                                  start=(kc == 0), stop=False), reads=[r_hT[b], r_win], writes=[psr[cb]], signal=False)
                          S.op("pe", lambda e, cb=cb: e.matmul(
                              ps[cb][:], lhsT=ones_b[0:1, :], rhs=brow[0:1, cb * 512:(cb + 1) * 512],
                              start=False, stop=True), reads=[r_const, r_brow], writes=[psr[cb]])
                      S.op("act", lambda e, tt=tt: e.activation(out=f_tm[:, tt, :], in_=ps[0][:, 0:256], func=AF.Identity),
                           reads=[psr[0]], writes=[r_f])
                      qb = qk_tm[b]
                      zst = xt[b]
                      rz = r_xt[b]
                      S.op("act", lambda e, zst=zst: e.activation(out=zst[:, 0:256], in_=ps[0][:, 256:512], func=AF.Identity),
                           reads=[psr[0]], writes=[rz])
                      S.op("act", lambda e, zst=zst: e.activation(out=zst[:, 256:768], in_=ps[1][:], func=AF.Identity),
                           reads=[psr[1]], writes=[rz])
                      S.op("act", lambda e, zst=zst: e.activation(out=zst[:, 768:1024], in_=ps[2][:, 0:256], func=AF.Identity),
                           reads=[psr[2]], writes=[rz])
                      S.op("act", lambda e, tt=tt: e.activation(
                          out=v_aug[:, tt, :, 0:64], in_=ps[2][:, 256:512].rearrange("p (h d) -> p h d", h=4),
                          func=AF.Identity), reads=[psr[2]], writes=[r_v])
                      cosb = ropec[:, tt, :].unsqueeze(1).to_broadcast([128, 16, 8])
                      sinb = ropes[:, tt, :].unsqueeze(1).to_broadcast([128, 16, 8])
                      zv = zst.rearrange("p (h d) -> p h d", h=16)
                      x1 = zv[:, :, 0:8]
                      x2 = zv[:, :, 8:16]
                      t0, t1, t2, t3 = (ropet[:, i, :, :] for i in range(4))
                      S.op("dve", lambda e, t0=t0, x1=x1, cosb=cosb: e.tensor_tensor(out=t0, in0=x1, in1=cosb, op=ALU.mult),
                           reads=[rz, r_const], writes=[r_ropet])
                      S.op("dve", lambda e, t1=t1, x2=x2, sinb=sinb: e.tensor_tensor(out=t1, in0=x2, in1=sinb, op=ALU.mult),
                           reads=[rz, r_const], writes=[r_ropet])
                      S.op("dve", lambda e, t2=t2, x2=x2, cosb=cosb: e.tensor_tensor(out=t2, in0=x2, in1=cosb, op=ALU.mult),
                           reads=[rz, r_const], writes=[r_ropet])
                      S.op("dve", lambda e, t3=t3, x1=x1, sinb=sinb: e.tensor_tensor(out=t3, in0=x1, in1=sinb, op=ALU.mult),
                           reads=[rz, r_const], writes=[r_ropet])
                      S.op("dve", lambda e, x1=x1, t0=t0, t1=t1: e.tensor_tensor(out=x1, in0=t0, in1=t1, op=ALU.subtract),
                           reads=[r_ropet], writes=[rz])
                      S.op("dve", lambda e, x2=x2, t2=t2, t3=t3: e.tensor_tensor(out=x2, in0=t2, in1=t3, op=ALU.add),
                           reads=[r_ropet], writes=[rz])
                      qdst = qb[:, 0:768].rearrange("p (h d) -> p h d", h=12)
                      for half6 in range(2):
                          src = zv[:, half6 * 6:(half6 + 1) * 6, :].rearrange("p (a b) d -> p a b d", a=2)
                          dst = qdst[:, half6 * 6:(half6 + 1) * 6, :].rearrange("p (b a) d -> p a b d", a=2)
                          S.op("pool", lambda e, src=src, dst=dst: e.tensor_copy(out=dst, in_=src), reads=[rz],
                               writes=[r_qk[b]])
                      S.op("act", lambda e, zst=zst, qb=qb: e.activation(out=qb[:, 768:1024], in_=zst[:, 768:1024],
                                                                         func=AF.Identity), reads=[rz], writes=[r_qk[b]])

                  def stC(tt, s=s, l=l):
                      gt = s * TPS + tt
                      b = tt % 2
                      qb = qk_tm[b]
                      pq = ps_bf(3).rearrange("p (c t) -> p c t", c=8)
                      for c in range(8):
                          S.op("pe", lambda e, c=c, qb=qb: e.transpose(out=pq[:, c, :], in_=qb[:, c * 128:(c + 1) * 128],
                                                                       identity=ident_b),
                               reads=[r_qk[b], r_const], writes=[psr[3]], signal=(c == 7))
                      S.op("act", lambda e, tt=tt: e.activation(out=QT[:, :, tt * 128:(tt + 1) * 128], in_=pq[:, 0:6, :],
                                                                func=AF.Identity), reads=[psr[3]], writes=[r_QT])
                      S.op("act", lambda e, tt=tt: e.activation(out=KT[:, :, tt * 128:(tt + 1) * 128], in_=pq[:, 6:8, :],
                                                                func=AF.Identity), reads=[psr[3]], writes=[r_KT])


                  for k_ in range(TPS + 3):
                      if k_ < TPS:
                          stA(k_)
                      if 0 <= k_ - 1 < TPS:
                          stA2(k_ - 1)
                      if 0 <= k_ - 2 < TPS:
                          stB(k_ - 2)
                      if 0 <= k_ - 3 < TPS:
                          stC(k_ - 3)

                  if stage == 'A1':
                      raise _Stop()
                  for kb in range(SEQ // 128):
                      b = kb % 2
                      S.dma("sp", lambda e, kb=kb, b=b: e.dma_start(
                          out=dftb[b][0], in_=dftc[kb].rearrange("p (t k) -> p t k", t=TPS)),
                          writes=[r_dft[b]])
                      S.dma("sp", lambda e, kb=kb, b=b: e.dma_start(
                          out=dftb[b][1], in_=dfts[kb].rearrange("p (t k) -> p t k", t=TPS)),
                          writes=[r_dft[b]])
                      for t in range(2):
                          for cc in range(2):
                              for stile in range(TPS):
                                  S.op("pe", lambda e, t=t, cc=cc, stile=stile, b=b: e.matmul(
                                      ps[4][:, (t * 2 + cc) * 128:(t * 2 + cc + 1) * 128],
                                      lhsT=f_tm[:, stile, cc * 128:(cc + 1) * 128], rhs=dftb[b][t][:, stile, :],
                                      start=(stile == 0), stop=(stile == TPS - 1)),
                                      reads=[r_f, r_dft[b]], writes=[psr[4]], signal=(stile == TPS - 1))
                      S.op("act", lambda e, b=b: e.activation(
                          out=ysb[b], in_=ps[4][:].rearrange("p (a c k) -> p a c k", a=2, c=2), func=AF.Identity),
                          reads=[psr[4]], writes=[r_ysb[b]])
                      for cc in range(2):
                          S.op("pe", lambda e, cc=cc, b=b: e.matmul(ps[5][:, cc * 128:(cc + 1) * 128], lhsT=ablk[:, cc, :],
                                                                   rhs=ysb[b][:, 0, cc, :], start=True, stop=False),
                               reads=[r_ablk, r_ysb[b]], writes=[psr[5]], signal=False)
                          S.op("pe", lambda e, cc=cc, b=b: e.matmul(ps[5][:, cc * 128:(cc + 1) * 128], lhsT=nbblk[:, cc, :],
                                                                   rhs=ysb[b][:, 1, cc, :], start=False, stop=True),
                               reads=[r_ablk, r_ysb[b]], writes=[psr[5]])
                      S.op("dve", lambda e, kb=kb: e.tensor_copy(
                          out=foT[:, :, kb * 128:(kb + 1) * 128], in_=ps[5][:, 0:256].rearrange("p (c k) -> p c k", c=2)),
                          reads=[psr[5]], writes=[r_foT])

                  if stage == 'A3':
                      raise _Stop()
                  def stX(n, s=s, l=l):
                      ab = n % 2
                      js = [j for j in (n - 1, n, n + 1) if 0 <= j < TPS]
                      ptsg = {}

                      def qk_exp(g):
                          off = (g % 2) * 64
                          kc_ = g // 2
                          pts = []
                          for ji, j in enumerate(js):
                              sbank = 3 + (stX.cnt % 2)
                              stX.cnt += 1
                              pt_i = (g % 2) * 3 + ji
                              for i in range(3):
                                  S.op("pe", lambda e, i=i, j=j, off=off, kc_=kc_, sbank=sbank, n=n: e.matmul(
                                      ps[sbank][:, i * 128:(i + 1) * 128],
                                      lhsT=KT[off:off + 64, kc_, j * 128:(j + 1) * 128],
                                      rhs=QT[off:off + 64, kc_ * 3 + i, n * 128:(n + 1) * 128], start=True, stop=True),
                                      reads=[r_KT, r_QT], writes=[psr[sbank]], signal=(i == 2))
                              ptile = PT[pt_i]
                              S.op("act", lambda e, ptile=ptile, sbank=sbank: e.activation(
                                  out=ptile, in_=ps[sbank][:, 0:384].rearrange("p (i q) -> p i q", i=3), func=AF.Exp,
                                  scale=0.125), reads=[psr[sbank]], writes=[r_PT[pt_i]])
                              if j != n:
                                  mk = maska_b if j < n else maskb_b
                                  S.op("dve", lambda e, ptile=ptile, mk=mk: e.tensor_tensor(
                                      out=ptile, in0=ptile, in1=mk.unsqueeze(1).to_broadcast([128, 3, 128]), op=ALU.mult),
                                      reads=[r_const], writes=[r_PT[pt_i]])
                              pts.append((j, ptile, r_PT[pt_i]))
                          ptsg[g] = pts

                      def pv(g):
                          pts = ptsg[g]
                          for i in range(3):
                              h = 3 * g + i
                              obank = 5 + h // 6
                              ocol = (h % 6) * 65
                              for ji, (j, ptile, rpt) in enumerate(pts):
                                  S.op("pe", lambda e, i=i, j=j, g=g, ptile=ptile, obank=obank, ocol=ocol, ji=ji,
                                       nj=len(pts): e.matmul(
                                      ps[obank][:, ocol:ocol + 65], lhsT=ptile[:, i, :], rhs=v_aug[:, j, g, 0:65],
                                      start=(ji == 0), stop=(ji == nj - 1)),
                                      reads=[rpt, r_v], writes=[psr[obank]], signal=(ji == len(pts) - 1))

                      for g in range(5):
                          if g < 4:
                              qk_exp(g)
                          if g >= 1:
                              pv(g - 1)
                      for hb2 in range(2):
                          ov = ps[5 + hb2][:, 0:390].rearrange("p (h d) -> p h d", h=6)
                          S.op("dve", lambda e, ov=ov, hb2=hb2: e.tensor_tensor(
                              out=den[:, hb2 * 6:(hb2 + 1) * 6].unsqueeze(2), in0=ov[:, :, 64:65],
                              in1=expsink[:, hb2 * 6:(hb2 + 1) * 6].unsqueeze(2), op=ALU.add),
                              reads=[psr[5 + hb2], r_sink], writes=[r_den])
                          S.op("dve", lambda e, hb2=hb2: e.reciprocal(out=den[:, hb2 * 6:(hb2 + 1) * 6],
                                                                      in_=den[:, hb2 * 6:(hb2 + 1) * 6]),
                               reads=[r_den], writes=[r_den])
                          S.op("dve", lambda e, ov=ov, hb2=hb2, ab=ab: e.tensor_tensor(
                              out=attn_tm[ab][:, hb2 * 6:(hb2 + 1) * 6, :], in0=ov[:, :, 0:64],
                              in1=den[:, hb2 * 6:(hb2 + 1) * 6].unsqueeze(2).to_broadcast([128, 6, 64]), op=ALU.mult),
                              reads=[psr[5 + hb2], r_den], writes=[r_attn[ab]])
                      av = attn_tm[ab].rearrange("p h d -> p (h d)")
                      pa = ps_bf(7).rearrange("p (c t) -> p c t", c=8)
                      for c in range(6):
                          S.op("pe", lambda e, c=c, av=av: e.transpose(out=pa[:, c, :], in_=av[:, c * 128:(c + 1) * 128],
                                                                       identity=ident_b),
                               reads=[r_attn[ab], r_const], writes=[psr[7]], signal=(c == 5))
                      S.op("act", lambda e, ab=ab: e.activation(out=attnT[ab], in_=pa[:, 0:6, :], func=AF.Identity),
                           reads=[psr[7]], writes=[r_attnT[ab]])

                  stX.cnt = 0

                  def stY(n, s=s, l=l):
                      gt = s * TPS + n
                      ab = n % 2
                      rsl = r_slot_t[gt]
                      for half in range(2):
                          for kc in range(8):
                              if kc < 2:
                                  lhs = foT[:, kc, n * 128:(n + 1) * 128]
                                  rd = [r_foT, r_wo]
                              else:
                                  lhs = attnT[ab][:, kc - 2, :]
                                  rd = [r_attnT[ab], r_wo]
                              S.op("pe", lambda e, half=half, kc=kc, lhs=lhs: e.matmul(
                                  ps[half][:], lhsT=lhs, rhs=w_o_sb[:, kc, half * 512:(half + 1) * 512],
                                  start=(kc == 0), stop=(kc == 7)), reads=rd, writes=[psr[half]], signal=(kc == 7))
                      mb_ = n % 2
                      S.dma("sp", lambda e, gt=gt, l=l: e.dma_start(out=xb, in_=xsrc(l)[gt * 128:(gt + 1) * 128, :]),
                            writes=[r_xb])
                      S.op("pool", lambda e: e.tensor_tensor(out=xb, in0=xb, in1=bcf[:, 3, :], op=ALU.add),
                           reads=[r_bc], writes=[r_xb])
                      for half in range(2):
                          S.op("dve", lambda e, half=half: e.tensor_tensor(
                              out=tmpf[:, half * 512:(half + 1) * 512], in0=ps[half][:],
                              in1=bcf[:, 2, half * 512:(half + 1) * 512], op=ALU.mult),
                              reads=[psr[half], r_bc], writes=[r_tmpf])
                      S.op("dve", lambda e, mb_=mb_: e.tensor_tensor(out=xmid[mb_], in0=tmpf, in1=xb, op=ALU.add),
                           reads=[r_tmpf, r_xb], writes=[r_xmid[mb_]])
                      if stage == "A":
                          S.dma("sp", lambda e, gt=gt, mb_=mb_: e.dma_start(out=y[gt * 128:(gt + 1) * 128, :], in_=xmid[mb_]),
                                reads=[r_xmid[mb_]])
                          return
                      S.dma("sp", lambda e, gt=gt, mb_=mb_: e.dma_start(out=xres[gt * 128:(gt + 1) * 128, :], in_=xmid[mb_]),
                            reads=[r_xmid[mb_]])
                      rms_mod(xmid[mb_], r_xmid[mb_], 4, 5, h2[mb_], r_h2[mb_])

                  def stY2(n, s=s, l=l):
                      gt = s * TPS + n
                      mb_ = n % 2
                      rsl = r_slot_t[gt]
                      transpose8(h2[mb_], r_h2[mb_], 0, h2T, r_h2T, "act")
                      for kc in range(8):
                          S.op("pe", lambda e, kc=kc: e.matmul(ps[2][:, 0:NE], lhsT=h2T[:, kc, :], rhs=wr_sb[:, kc, :],
                                                               start=(kc == 0), stop=False),
                               reads=[r_h2T, r_wr], writes=[psr[2]], signal=False)
                      S.op("pe", lambda e: e.matmul(ps[2][:, 0:NE], lhsT=ones_b[0:1, :], rhs=brow[0:1, INW:INW + NE],
                                                    start=False, stop=True), reads=[r_const, r_brow], writes=[psr[2]])
                      R = [r_rt]
                      S.op("dve", lambda e: e.tensor_copy(out=rt_lg, in_=ps[2][:, 0:NE]), reads=[psr[2]], writes=R)
                      S.op("dve", lambda e: e.max(out=rt_t8, in_=rt_lg), writes=R)
                      S.op("dve", lambda e: e.tensor_scalar(out=rt_mask, in0=rt_lg, scalar1=rt_t8[:, 3:4], scalar2=None,
                                                            op0=ALU.is_ge), writes=R)
                      S.op("dve", lambda e: e.tensor_copy(out=rt_maskb, in_=rt_mask), writes=R)
                      S.op("dve", lambda e: e.tensor_scalar(out=rt_small[:, 0:1], in0=rt_t8[:, 0:1], scalar1=-1.0,
                                                            scalar2=None, op0=ALU.mult), writes=R)
                      S.op("act", lambda e: e.activation(out=rt_e, in_=rt_lg, func=AF.Exp, bias=rt_small[:, 0:1]),
                           reads=R, writes=R)
                      S.op("dve", lambda e: e.scalar_tensor_tensor(out=rt_G, in0=rt_e, scalar=1.0, in1=rt_mask,
                                                                   op0=ALU.mult, op1=ALU.mult,
                                                                   accum_out=rt_small[:, 1:2]), reads=R, writes=R)
                      S.op("dve", lambda e: e.reciprocal(out=rt_small[:, 2:3], in_=rt_small[:, 1:2]), writes=R)
                      S.op("dve", lambda e: e.tensor_scalar(out=rt_G, in0=rt_G, scalar1=rt_small[:, 2:3], scalar2=None,
                                                            op0=ALU.mult), writes=R)
                      S.op("pe", lambda e: e.matmul(ps[2][:, 64:64 + NE], lhsT=lstrict_b, rhs=rt_maskb, start=True, stop=True),
                           reads=[r_rt, r_const], writes=[psr[2]])
                      S.op("pe", lambda e: e.matmul(ps[2][:, 128:128 + NE], lhsT=ones_b, rhs=rt_maskb, start=True, stop=True),
                           reads=[r_rt, r_const], writes=[psr[2]])
                      S.op("dve", lambda e: e.tensor_tensor(out=rt_cnt, in0=ps[2][:, 64:64 + NE], in1=run_cnt, op=ALU.add),
                           reads=[psr[2], r_run], writes=R)
                      S.op("dve", lambda e: e.tensor_tensor(out=run_cnt, in0=ps[2][:, 128:128 + NE], in1=run_cnt, op=ALU.add),
                           reads=[psr[2]], writes=[r_run])
                      S.op("dve", lambda e: e.scalar_tensor_tensor(out=rt_ms, in0=rt_cnt, scalar=float(C), in1=rt_mask,
                                                                   op0=ALU.is_lt, op1=ALU.mult), writes=R)
                      S.op("dve", lambda e: e.tensor_tensor(out=rt_G, in0=rt_G, in1=rt_ms, op=ALU.mult), writes=R)
                      S.op("dve", lambda e: e.scalar_tensor_tensor(out=rt_cnt, in0=rt_cnt, scalar=1.0, in1=ebase,
                                                                   op0=ALU.add, op1=ALU.add), reads=[r_const], writes=R)
                      S.op("dve", lambda e: e.tensor_tensor(out=rt_ms, in0=rt_ms, in1=rt_cnt, op=ALU.mult), writes=R)
                      S.op("dve", lambda e: e.max(out=rt_s8, in_=rt_ms), writes=R)
                      for k in range(4):
                          S.op("dve", lambda e, k=k, gt=gt: e.scalar_tensor_tensor(
                              out=rt_junk, in0=rt_ms, scalar=rt_s8[:, k:k + 1], in1=rt_G, op0=ALU.is_equal, op1=ALU.mult,
                              accum_out=gate_all[:, gt, k:k + 1]), writes=R + [rsl])
                      S.op("dve", lambda e, gt=gt: e.tensor_scalar(out=slot_g_all[:, gt, :], in0=rt_s8[:, 0:4], scalar1=1.0,
                                                                   scalar2=-1.0, op0=ALU.max, op1=ALU.add),
                           writes=R + [rsl])
                      S.op("dve", lambda e: e.tensor_scalar(out=rt_small[:, 12:16], in0=rt_s8[:, 0:4], scalar1=0.5,
                                                            scalar2=None, op0=ALU.is_lt), writes=R)
                      S.op("dve", lambda e, gt=gt: e.scalar_tensor_tensor(
                          out=rt_small[:, 4:8], in0=trashc, scalar=float((gt % 8) * 512), in1=rt_small[:, 12:16],
                          op0=ALU.add, op1=ALU.mult), reads=[r_const], writes=R)
                      S.op("dve", lambda e: e.scalar_tensor_tensor(out=rt_small[:, 8:12], in0=rt_s8[:, 0:4], scalar=-1.0,
                                                                   in1=rt_small[:, 4:8], op0=ALU.add, op1=ALU.add), writes=R)
                      S.op("dve", lambda e, gt=gt: e.tensor_copy(out=slot_sc_all[:, gt, :], in_=rt_small[:, 8:12]),
                           writes=R + [rsl])
                      for k in range(4):
                          S.dma("pool", lambda e, k=k, gt=gt, mb_=mb_: e.indirect_dma_start(
                              out=Xs, out_offset=bass.IndirectOffsetOnAxis(ap=slot_sc_t[:, gt * 4 + k:gt * 4 + k + 1], axis=0),
                              in_=h2[mb_], in_offset=None),
                              reads=[rsl, r_h2[mb_]])

                  for k_ in range(TPS + 2):
                      if k_ < TPS:
                          stX(k_)
                      if 0 <= k_ - 1 < TPS:
                          stY(k_ - 1)
                      if 0 <= k_ - 2 < TPS and stage != "A":
                          stY2(k_ - 2)
              S.op("dve", lambda e, l=l: e.tensor_copy(out=dbg_sb[:, l * NE:(l + 1) * NE], in_=run_cnt),
                   reads=[r_run], writes=[r_dbg])
              S.barrier()
              AR.release(mA)
              if stage == "A":
                  continue

              mB = AR.mark()
              wgu = [AR.alloc([8, 2 * DFF], BF16) for _ in range(2)]
              wdn = [AR.alloc([4, D], BF16) for _ in range(2)]
              bdb = [AR.alloc([D]) for _ in range(2)]
              r_w = [Res(), Res()]
              xs = [AR.alloc([4, D], BF16) for _ in range(2)]
              r_xs = [Res(), Res()]
              XT = [AR.alloc([8, 512], BF16) for _ in range(2)]
              r_XT = [Res(), Res()]
              gcb = [AR.alloc([512]) for _ in range(2)]
              uab = [AR.alloc([512]) for _ in range(2)]
              sgb = [AR.alloc([512]) for _ in range(2)]
              ucb = [AR.alloc([512]) for _ in range(2)]
              tb = [AR.alloc([512]) for _ in range(2)]
              r_gc, r_ua, r_sg, r_uc, r_tb = ([Res(), Res()] for _ in range(5))
              actT = [AR.alloc([4, 512], BF16) for _ in range(2)]
              r_act = [Res(), Res()]
              ysbuf = [AR.alloc([4, D], BF16) for _ in range(2)]
              r_ys = [Res(), Res()]

              def load_w(ex, l=l):
                  wb = ex % 2
                  S.dma("pool", lambda e, l=l, ex=ex, wb=wb: e.dma_start(
                      out=wgu[wb], in_=w_gu[l, ex].rearrange("(c p) f -> p c f", p=128)), writes=[r_w[wb]])
                  S.dma("pool", lambda e, l=l, ex=ex, wb=wb: e.dma_start(
                      out=wdn[wb], in_=w_down[l, ex].rearrange("(c p) f -> p c f", p=128)), writes=[r_w[wb]])
                  S.dma("sp", lambda e, l=l, ex=ex, wb=wb: e.dma_start(
                      out=bdb[wb], in_=b_down[l, ex:ex + 1, :].partition_broadcast(128)), writes=[r_w[wb]])

              items = [(ex, g) for ex in range(NE) for g in range(NG)]

              def stT(i):
                  ex, g = items[i]
                  r0 = ex * C + g * 512
                  ib = i % 2
                  S.dma("sp", lambda e, r0=r0, ib=ib: e.dma_start(
                      out=xs[ib], in_=Xs[r0:r0 + 512, :].rearrange("(t p) d -> p t d", p=128)), writes=[r_xs[ib]])
                  for rt in range(4):
                      bank = 6 + (rt % 2)
                      pv = ps_bf(bank).rearrange("p (c t) -> p c t", c=8)
                      for c in range(8):
                          S.op("pe", lambda e, c=c, rt=rt, ib=ib, pv=pv: e.transpose(
                              out=pv[:, c, :], in_=xs[ib][:, rt, c * 128:(c + 1) * 128], identity=ident_b),
                              reads=[r_xs[ib], r_const], writes=[psr[bank]], signal=(c == 7))
                      if rt % 2 == 0:
                          S.op("act", lambda e, rt=rt, pv=pv, ib=ib: e.activation(
                              out=XT[ib][:, :, rt * 128:(rt + 1) * 128], in_=pv, func=AF.Identity),
                              reads=[psr[bank]], writes=[r_XT[ib]])
                      else:
                          S.op("dve", lambda e, rt=rt, pv=pv, ib=ib: e.tensor_copy(
                              out=XT[ib][:, :, rt * 128:(rt + 1) * 128], in_=pv),
                              reads=[psr[bank]], writes=[r_XT[ib]])

              def stG(i):
                  ex, g = items[i]
                  wb = ex % 2
                  ib = i % 2
                  for pr in range(4):
                      q2 = pr % 2
                      gbank = (pr % 2) * 2
                      ubank = gbank + 1
                      for (fo, bank) in ((pr, gbank), (pr + 4, ubank)):
                          for kc in range(8):
                              S.op("pe", lambda e, fo=fo, bank=bank, kc=kc, wb=wb, ib=ib: e.matmul(
                                  ps[bank][:], lhsT=wgu[wb][:, kc, fo * 128:(fo + 1) * 128], rhs=XT[ib][:, kc, :],
                                  start=(kc == 0), stop=(kc == 7)), reads=[r_w[wb], r_XT[ib]], writes=[psr[bank]],
                                  signal=(kc == 7))
                      S.op("act", lambda e, pr=pr, ex=ex, ubank=ubank, q2=q2: e.activation(
                          out=uab[q2], in_=ps[ubank][:], func=AF.Identity, bias=bguT[:, pr + 4, ex:ex + 1]),
                          reads=[psr[ubank], r_bgu], writes=[r_ua[q2]])
                      S.op("dve", lambda e, pr=pr, ex=ex, gbank=gbank, q2=q2: e.tensor_scalar(
                          out=gcb[q2], in0=ps[gbank][:], scalar1=bguT[:, pr, ex:ex + 1], scalar2=7.0, op0=ALU.add,
                          op1=ALU.min), reads=[psr[gbank], r_bgu], writes=[r_gc[q2]])
                      S.op("act", lambda e, q2=q2: e.activation(out=sgb[q2], in_=gcb[q2], func=AF.Sigmoid, scale=1.702),
                           reads=[r_gc[q2]], writes=[r_sg[q2]])
                      S.op("pool", lambda e, q2=q2: e.tensor_scalar(out=ucb[q2], in0=uab[q2], scalar1=7.0, scalar2=-7.0,
                                                                    op0=ALU.min, op1=ALU.max),
                           reads=[r_ua[q2]], writes=[r_uc[q2]])
                      S.op("dve", lambda e, q2=q2: e.tensor_tensor(out=tb[q2], in0=gcb[q2], in1=sgb[q2], op=ALU.mult),
                           reads=[r_gc[q2], r_sg[q2]], writes=[r_tb[q2]])
                      S.op("dve", lambda e, pr=pr, ib=ib, q2=q2: e.scalar_tensor_tensor(
                          out=actT[ib][:, pr, :], in0=ucb[q2], scalar=1.0, in1=tb[q2], op0=ALU.add, op1=ALU.mult),
                          reads=[r_uc[q2], r_tb[q2]], writes=[r_act[ib]])

              def stD(i):
                  ex, g = items[i]
                  r0 = ex * C + g * 512
                  wb = ex % 2
                  ib = i % 2
                  for rt in range(4):
                      for half in range(2):
                          bank = 4 + half
                          for fc in range(4):
                              S.op("pe", lambda e, rt=rt, half=half, fc=fc, ib=ib, wb=wb, bank=bank: e.matmul(
                                  ps[bank][:], lhsT=actT[ib][:, fc, rt * 128:(rt + 1) * 128],
                                  rhs=wdn[wb][:, fc, half * 512:(half + 1) * 512], start=(fc == 0), stop=(fc == 3)),
                                  reads=[r_act[ib], r_w[wb]], writes=[psr[bank]], signal=(fc == 3))
                          S.op("dve", lambda e, rt=rt, half=half, ib=ib, wb=wb, bank=bank: e.tensor_tensor(
                              out=ysbuf[ib][:, rt, half * 512:(half + 1) * 512], in0=ps[bank][:],
                              in1=bdb[wb][:, half * 512:(half + 1) * 512], op=ALU.add),
                              reads=[psr[bank], r_w[wb]], writes=[r_ys[ib]])
                  S.dma("sp", lambda e, r0=r0, ib=ib: e.dma_start(
                      out=Yd[r0:r0 + 512, :].rearrange("(t p) d -> p t d", p=128), in_=ysbuf[ib]), reads=[r_ys[ib]])

              load_w(0)
              stT(0)
              for i in range(len(items)):
                  ex, g = items[i]
                  if g == 0 and ex + 1 < NE:
                      load_w(ex + 1)
                  stG(i)
                  if i + 1 < len(items):
                      stT(i + 1)
                  stD(i)
              S.barrier()
              AR.release(mB)

              mC = AR.mark()
              DEPTH = 6
              g2b = [AR.alloc([D]) for _ in range(2)]
              r_g2 = [Res(), Res()]
              fgb = AR.alloc([D])
              r_fg = Res()
              xm = [AR.alloc([D]) for _ in range(DEPTH)]
              r_xm = [Res() for _ in range(DEPTH)]
              yk = [[AR.alloc([D], BF16) for _ in range(4)] for _ in range(DEPTH)]
              r_yk = [Res() for _ in range(DEPTH)]
              acc = [AR.alloc([D]) for _ in range(DEPTH)]
              r_acc = [Res() for _ in range(DEPTH)]
              sq2 = AR.alloc([D], BF16)
              r_sq2 = Res()
              st2 = AR.alloc([8])
              r_st2 = Res()
              if last_layer:
                  S.dma("sp", lambda e: e.dma_start(out=fgb, in_=final_g.partition_broadcast(128)), writes=[r_fg])

              def cL(gt):
                  s_ = gt // TPS
                  b = gt % DEPTH
                  if gt % TPS == 0:
                      S.dma("sp", lambda e, s_=s_: e.dma_start(out=g2b[s_ % 2],
                                                                in_=modscr[s_, 6:7, :].partition_broadcast(128)),
                            writes=[r_g2[s_ % 2]])
                  S.dma("sp", lambda e, gt=gt, b=b: e.dma_start(out=xm[b], in_=xres[gt * 128:(gt + 1) * 128, :]),
                        writes=[r_xm[b]])
                  for k in range(4):
                      S.dma("pool", lambda e, gt=gt, b=b, k=k: e.indirect_dma_start(
                          out=yk[b][k], out_offset=None, in_=Yd,
                          in_offset=bass.IndirectOffsetOnAxis(ap=slot_g_t[:, gt * 4 + k:gt * 4 + k + 1], axis=0)),
                          reads=[r_slot_t[gt]], writes=[r_yk[b]])

              def cC(gt):
                  s_ = gt // TPS
                  b = gt % DEPTH
                  gb = g2b[s_ % 2]
                  S.op("dve", lambda e, gt=gt, b=b: e.tensor_scalar(out=acc[b], in0=yk[b][0], scalar1=gate_all[:, gt, 0:1],
                                                                    scalar2=None, op0=ALU.mult),
                       reads=[r_yk[b], r_slot_t[gt]], writes=[r_acc[b]])
                  for k in range(1, 4):
                      S.op("dve", lambda e, gt=gt, b=b, k=k: e.scalar_tensor_tensor(
                          out=acc[b], in0=yk[b][k], scalar=gate_all[:, gt, k:k + 1], in1=acc[b], op0=ALU.mult, op1=ALU.add),
                          reads=[r_yk[b], r_slot_t[gt]], writes=[r_acc[b]])
                  S.op("dve", lambda e, b=b, gb=gb: e.tensor_tensor(out=acc[b], in0=acc[b], in1=gb, op=ALU.mult),
                       reads=[r_g2[s_ % 2]], writes=[r_acc[b]])
                  S.op("dve", lambda e, b=b: e.tensor_tensor(out=xm[b], in0=acc[b], in1=xm[b], op=ALU.add),
                       reads=[r_acc[b]], writes=[r_xm[b]])
                  if last_layer:
                      S.op("act", lambda e, b=b: e.activation(out=sq2, in_=xm[b], func=AF.Square, accum_out=st2[:, 0:1]),
                           reads=[r_xm[b]], writes=[r_sq2, r_st2])
                      S.op("act", lambda e: e.activation(out=st2[:, 1:2], in_=st2[:, 0:1], func=AF.Sqrt, scale=1.0 / D,
                                                         bias=EPS), reads=[r_st2], writes=[r_st2])
                      S.op("dve", lambda e: e.reciprocal(out=st2[:, 2:3], in_=st2[:, 1:2]), reads=[r_st2], writes=[r_st2])
                      S.op("dve", lambda e, b=b: e.scalar_tensor_tensor(out=xm[b], in0=xm[b], scalar=st2[:, 2:3],
                                                                        in1=fgb, op0=ALU.mult, op1=ALU.mult),
                           reads=[r_st2, r_fg], writes=[r_xm[b]])
                  S.dma("sp", lambda e, gt=gt, b=b: e.dma_start(out=y[gt * 128:(gt + 1) * 128, :], in_=xm[b]),
                        reads=[r_xm[b]])

              LA = DEPTH - 1
              for i_ in range(NT + LA):
                  if i_ < NT:
                      cL(i_)
                  if i_ >= LA:
                      cC(i_ - LA)
              S.barrier()
              AR.release(mC)

          except _Stop:
            S.barrier()
            break
        if stage == "full":
            S.dma("sp", lambda e: e.dma_start(out=dbg, in_=dbg_sb), reads=[r_dbg])
        S.final_wait("sp")
        S.emit()
        print("instructions:", S.n_inst, flush=True)
    return nc


def make_consts():
    bf = ml_dtypes.bfloat16
    s = np.arange(SEQ, dtype=np.int64)
    ph = (np.outer(s, s) % SEQ).astype(np.float64) * (2.0 * np.pi / SEQ)
    def blk(m):
        m = m.astype(np.float32).reshape(TPS, 128, TPS, 128).transpose(2, 1, 0, 3)
        return np.ascontiguousarray(m.reshape(TPS, 128, TPS * 128)).astype(bf)
    dftc = blk(np.cos(ph))
    dfts = blk(np.sin(ph))
    c = np.arange(64, dtype=np.int64)
    ph64 = (np.outer(c, c) % 64).astype(np.float64) * (2.0 * np.pi / 64)
    nrm = 1.0 / np.sqrt(SEQ * 64.0)
    cc = np.cos(ph64) * nrm
    sc = np.sin(ph64) * nrm
    ccb = np.zeros((128, 128), np.float32)
    scb = np.zeros((128, 128), np.float32)
    for g in range(2):
        ccb[g * 64:(g + 1) * 64, g * 64:(g + 1) * 64] = cc
        scb[g * 64:(g + 1) * 64, g * 64:(g + 1) * 64] = sc
    inv_freq = np.power(np.float32(500000.0), -np.arange(0, 16, 2, dtype=np.float32) / np.float32(16))
    ang = np.arange(SEQ, dtype=np.float32)[:, None] * inv_freq[None, :]
    a = np.arange(128)
    consts = {
        "dftc": dftc, "dfts": dfts, "ccb": ccb, "scb": scb,
        "rope_c": np.cos(ang).astype(np.float32), "rope_s": np.sin(ang).astype(np.float32),
        "k_ident": np.eye(128, dtype=np.float32),
        "k_lstrict": (a[:, None] < a[None, :]).astype(np.float32),
        "k_maska": (a[None, :] <= a[:, None]).astype(np.float32),
        "k_maskb": (a[:, None] <= a[None, :]).astype(np.float32),
    }
    return consts


_NC_CACHE = {}


def run(x_all, c_all, weights, NL, NSEQ, C, n_cores, stage="full"):
    key = (NL, NSEQ, C, stage)
    if key not in _NC_CACHE:
        _NC_CACHE[key] = build(NL, NSEQ, C, stage)
    nc = _NC_CACHE[key]
    consts = make_consts()
    consts["k_ebase"] = np.tile((np.arange(NE, dtype=np.float32) * C)[None, :], (128, 1)).astype(np.float32)
    consts["k_trash"] = (NE * C + 1 + np.arange(128, dtype=np.float32)[:, None]
                         + 128.0 * np.arange(4, dtype=np.float32)[None, :]).astype(np.float32)
    shared = dict(consts)
    for k, v in weights.items():
        v = np.ascontiguousarray(np.asarray(v, dtype=np.float32))
        if k == "final_g":
            v = v.reshape(1, D)
        else:
            v = v[:NL]
        shared[k] = v
    in_maps = []
    for ci in range(n_cores):
        m = dict(shared)
        xs = x_all[ci * NSEQ:(ci + 1) * NSEQ]
        m["x_in"] = np.ascontiguousarray(xs.reshape(NSEQ * SEQ, D))
        m["cT"] = np.ascontiguousarray(c_all[ci * NSEQ:(ci + 1) * NSEQ].T)
        in_maps.append(m)
    res = run_bass_kernel_spmd(nc, in_maps, core_ids=list(range(n_cores)))
    outs = [np.asarray(r["y"]).reshape(NSEQ, SEQ, D) for r in res.results]
    if stage == "full":
        try:
            cnts = np.stack([np.asarray(r["dbg"])[0].reshape(NL, NE) for r in res.results])
            print("expert-count max per layer (capacity %d):" % C, cnts.max(axis=(0, 2)).tolist(),
                  "overflow assignments per layer:", np.maximum(cnts - C, 0).sum(axis=(0, 2)).tolist(), flush=True)
        except Exception as ex:
            print("dbg unavailable", ex)
    return np.concatenate(outs, axis=0)


CAPACITY = 3072


def kernel(x_prompt, x_sample, c_prompt, c_sample, norm1_g, ada_w, ada_b, w_in, b_in, w_fmix, sinks, w_o, b_o,
           norm2_g, w_router, b_router, w_gu, b_gu, w_down, b_down, final_g):
    x_prompt = np.asarray(x_prompt, dtype=np.float32)
    x_sample = np.asarray(x_sample, dtype=np.float32)
    c_prompt = np.asarray(c_prompt, dtype=np.float32)
    c_sample = np.asarray(c_sample, dtype=np.float32)
    n_cores = 8
    pp = x_prompt.shape[0] // n_cores
    sp = x_sample.shape[0] // n_cores
    xs, cs = [], []
    for ci in range(n_cores):
        xs.append(x_prompt[ci * pp:(ci + 1) * pp])
        xs.append(x_sample[ci * sp:(ci + 1) * sp])
        cs.append(c_prompt[ci * pp:(ci + 1) * pp])
        cs.append(c_sample[ci * sp:(ci + 1) * sp])
    x_all = np.concatenate(xs, axis=0)
    c_all = np.concatenate(cs, axis=0)
    weights = dict(norm1_g=norm1_g, ada_w=ada_w, ada_b=ada_b, w_in=w_in, b_in=b_in, w_fmix=w_fmix, sinks=sinks,
                   w_o=w_o, b_o=b_o, norm2_g=norm2_g, w_router=w_router, b_router=b_router, w_gu=w_gu, b_gu=b_gu,
                   w_down=w_down, b_down=b_down, final_g=final_g)
    out = run(x_all, c_all, weights, 4, pp + sp, CAPACITY, n_cores)
    nseq = pp + sp
    yp = np.concatenate([out[ci * nseq:ci * nseq + pp] for ci in range(n_cores)], axis=0)
    ys = np.concatenate([out[ci * nseq + pp:(ci + 1) * nseq] for ci in range(n_cores)], axis=0)
    return (np.ascontiguousarray(yp, dtype=np.float32), np.ascontiguousarray(ys, dtype=np.float32))
```
